# Optimizing a Trainium2 kernel written in Bass

```python
import math
import jax, jax.numpy as jnp
from jax import lax
import numpy as np

D_MODEL = 2048
BATCH = 1
SEQ = 8192
DEPTH = 4

N_MIXERS = 4
N_PER_MIXER = DEPTH // N_MIXERS
D_FF = 4 * D_MODEL
NORM_EPS = 1e-6
LN_EPS = 1e-5
CONV_WIDTH = 31
POOL_WINDOWS = (2, 4, 8, 16)
POOL_GROUP = D_MODEL // len(POOL_WINDOWS)
N_HEADS = 16
HEAD_DIM = D_MODEL // N_HEADS
N_KV_HEADS = 4
Q_PER_KV = N_HEADS // N_KV_HEADS
IDX_HEADS = 16
IDX_DIM = 64
TOPK_MAX = 256
Q_BLOCK = 128
ROPE_THETA = 500000.0
Q_WIDTH = N_HEADS * HEAD_DIM
KV_WIDTH = N_KV_HEADS * HEAD_DIM
QI_WIDTH = IDX_HEADS * IDX_DIM
ATTN_IN_WIDTH = Q_WIDTH + 2 * KV_WIDTH + QI_WIDTH + IDX_DIM + IDX_HEADS
ATTN_SPLITS = (Q_WIDTH, Q_WIDTH + KV_WIDTH, Q_WIDTH + 2 * KV_WIDTH,
               Q_WIDTH + 2 * KV_WIDTH + QI_WIDTH, Q_WIDTH + 2 * KV_WIDTH + QI_WIDTH + IDX_DIM)
SSM_GROUP = 16
SSM_GROUPS = D_MODEL // SSM_GROUP
SSM_STATE = 64

kernel_name = 'hybrid_interleaved_conv_pool_dsa_s5'


def _rmsnorm(x, g):
    xf = x.astype(jnp.float32)
    y = xf * lax.rsqrt(jnp.mean(xf * xf, axis=-1, keepdims=True) + NORM_EPS)
    return (y * g.astype(jnp.float32)).astype(x.dtype)


def _layernorm(x, g, b):
    xf = x.astype(jnp.float32)
    mu = jnp.mean(xf, axis=-1, keepdims=True)
    xc = xf - mu
    var = jnp.mean(xc * xc, axis=-1, keepdims=True)
    y = xc * lax.rsqrt(var + LN_EPS) * g.astype(jnp.float32) + b.astype(jnp.float32)
    return y.astype(x.dtype)


def _rope_partial(x, pos):
    d = x.shape[-1]
    r = d // 4
    half = r // 2
    inv = jnp.power(ROPE_THETA, -2.0 * jnp.arange(half, dtype=jnp.float32) / r)
    ang = pos.astype(jnp.float32)[:, None] * inv[None, :]
    cos = jnp.cos(ang)[:, None, :].astype(x.dtype)
    sin = jnp.sin(ang)[:, None, :].astype(x.dtype)
    x1 = x[..., :half]
    x2 = x[..., half:r]
    return jnp.concatenate([x1 * cos - x2 * sin, x2 * cos + x1 * sin, x[..., r:]], axis=-1)


def _conv_module(x, w_in, b_in, w_dw, b_dw, ln_g, ln_b, w_out):
    h = x @ w_in + b_in
    a, g = jnp.split(h, 2, axis=-1)
    h = a * jax.nn.sigmoid(g)
    h = lax.conv_general_dilated(
        h, w_dw[:, None, :], window_strides=(1,), padding=[(CONV_WIDTH - 1, 0)],
        dimension_numbers=('NWC', 'WIO', 'NWC'), feature_group_count=D_MODEL) + b_dw
    h = jax.nn.silu(_layernorm(h, ln_g, ln_b))
    return h @ w_out


def _pool_mixer(x, pool_w, pool_scale):
    bsz, L, _ = x.shape
    xf = x.astype(jnp.float32)
    cs = jnp.concatenate([jnp.zeros((bsz, 1, D_MODEL), jnp.float32), lax.cumsum(xf, axis=1)], axis=1)
    t1 = jnp.arange(1, L + 1, dtype=jnp.float32)[None, :, None]
    outs = []
    for g, w in enumerate(POOL_WINDOWS):
        sl = slice(g * POOL_GROUP, (g + 1) * POOL_GROUP)
        csg = cs[:, :, sl]
        lower = jnp.concatenate([jnp.zeros((bsz, w - 1, POOL_GROUP), jnp.float32), csg[:, :L - w + 1]], axis=1)
        mean = (csg[:, 1:] - lower) / jnp.minimum(t1, float(w))
        outs.append(mean - xf[:, :, sl])
    pooled = jnp.stack(outs, axis=2).astype(x.dtype)
    mixed = jnp.einsum('blgc,gce->blge', pooled, pool_w).reshape(bsz, L, D_MODEL)
    return mixed * pool_scale


def _sparse_attention(x, w_in, w_out):
    bsz, L, _ = x.shape
    proj = x @ w_in
    q, k, v, qi, ki, wi = jnp.split(proj, ATTN_SPLITS, axis=-1)
    pos = jnp.arange(L)
    q = _rope_partial(q.reshape(bsz, L, N_HEADS, HEAD_DIM), pos)
    k = _rope_partial(k.reshape(bsz, L, N_KV_HEADS, HEAD_DIM), pos)
    v = v.reshape(bsz, L, N_KV_HEADS, HEAD_DIM)
    qi = _rope_partial(qi.reshape(bsz, L, IDX_HEADS, IDX_DIM), pos)
    ki = _rope_partial(ki.reshape(bsz, L, 1, IDX_DIM), pos)[:, :, 0]
    wi = wi * (IDX_HEADS ** -0.5 * IDX_DIM ** -0.5)
    topk = min(TOPK_MAX, L // 4)
    nblk = L // Q_BLOCK

    def to_blocks(a):
        return jnp.moveaxis(a.reshape(bsz, nblk, Q_BLOCK, *a.shape[2:]), 1, 0)

    def block(args):
        qb, qib, wib, start = args
        tq = start + jnp.arange(Q_BLOCK)
        sc = jnp.einsum('bqhd,bsd->bqhs', qib, ki).astype(jnp.float32)
        I = jnp.einsum('bqhs,bqh->bqs', jax.nn.relu(sc), wib.astype(jnp.float32))
        causal = pos[None, :] <= tq[:, None]
        I = jnp.where(causal[None], I, -jnp.inf)
        _, idx = lax.top_k(I, topk)
        valid = idx <= tq[None, :, None]
        kg = jax.vmap(lambda kb, ib: kb[ib])(k, idx)
        vg = jax.vmap(lambda vb, ib: vb[ib])(v, idx)
        qg = qb.reshape(bsz, Q_BLOCK, N_KV_HEADS, Q_PER_KV, HEAD_DIM)
        s = jnp.einsum('bqngd,bqknd->bqngk', qg, kg).astype(jnp.float32) * (HEAD_DIM ** -0.5)
        s = jnp.where(valid[:, :, None, None, :], s, -jnp.inf)
        p = jax.nn.softmax(s, axis=-1).astype(vg.dtype)
        o = jnp.einsum('bqngk,bqknd->bqngd', p, vg)
        return o.reshape(bsz, Q_BLOCK, Q_WIDTH)

    out = lax.map(block, (to_blocks(q), to_blocks(qi), to_blocks(wi), jnp.arange(nblk) * Q_BLOCK))
    out = jnp.moveaxis(out, 0, 1).reshape(bsz, L, Q_WIDTH)
    return out @ w_out


def _complex_affine_combine(e1, e2):
    a1r, a1i, b1r, b1i = e1
    a2r, a2i, b2r, b2i = e2
    ar = a2r * a1r - a2i * a1i
    ai = a2r * a1i + a2i * a1r
    br = a2r * b1r - a2i * b1i + b2r
    bi = a2r * b1i + a2i * b1r + b2i
    return (ar, ai, br, bi)


def _s5_mixer(u, lam_re, lam_im, log_dt, b_re, b_im, c_re, c_im, d_skip, w_glu):
    bsz, L, _ = u.shape
    uf = u.astype(jnp.float32)
    ug = uf.reshape(bsz, L, SSM_GROUPS, SSM_GROUP)
    lr = lam_re.astype(jnp.float32)
    li = lam_im.astype(jnp.float32)
    dt = jnp.exp(log_dt.astype(jnp.float32))[:, None]
    mag = jnp.exp(lr * dt)
    ar = mag * jnp.cos(li * dt)
    ai = mag * jnp.sin(li * dt)
    den = lr * lr + li * li
    cr = ((ar - 1.0) * lr + ai * li) / den
    ci = (ai * lr - (ar - 1.0) * li) / den
    br = b_re.astype(jnp.float32)
    bi = b_im.astype(jnp.float32)
    bbr = cr[..., None] * br - ci[..., None] * bi
    bbi = cr[..., None] * bi + ci[..., None] * br
    bu_r = jnp.einsum('blgc,gpc->blgp', ug, bbr)
    bu_i = jnp.einsum('blgc,gpc->blgp', ug, bbi)
    a_r = jnp.broadcast_to(ar, bu_r.shape)
    a_i = jnp.broadcast_to(ai, bu_i.shape)
    _, _, h_r, h_i = lax.associative_scan(_complex_affine_combine, (a_r, a_i, bu_r, bu_i), axis=1)
    y = (jnp.einsum('blgp,gcp->blgc', h_r, c_re.astype(jnp.float32))
         - jnp.einsum('blgp,gcp->blgc', h_i, c_im.astype(jnp.float32)))
    y = y.reshape(bsz, L, D_MODEL) + d_skip.astype(jnp.float32) * uf
    y = jax.nn.gelu(y).astype(u.dtype)
    z = y @ w_glu
    a, g = jnp.split(z, 2, axis=-1)
    return a * jax.nn.sigmoid(g)


def _sq_relu_mlp(x, w_up, w_down):
    h = jax.nn.relu(x @ w_up)
    return (h * h) @ w_down


def setup_inputs(seed: int = 0) -> dict:
    key = jax.random.key(seed)
    ks = jax.random.split(key, 24)
    f32 = jnp.float32

    def nrm(k, shape, scale):
        return scale * jax.random.normal(k, shape, f32)

    P = N_PER_MIXER
    x = nrm(ks[0], (BATCH, SEQ, D_MODEL), 1.0)
    norm_gains = 1.0 + nrm(ks[1], (DEPTH, 4, D_MODEL), 0.05)
    mlp_w_up = nrm(ks[2], (DEPTH, D_MODEL, D_FF), D_MODEL ** -0.5)
    mlp_w_down = nrm(ks[3], (DEPTH, D_FF, D_MODEL), D_FF ** -0.5)
    conv_w_in = nrm(ks[4], (P, D_MODEL, 2 * D_MODEL), D_MODEL ** -0.5)
    conv_b_in = nrm(ks[5], (P, 2 * D_MODEL), 0.02)
    conv_w_dw = nrm(ks[6], (P, CONV_WIDTH, D_MODEL), CONV_WIDTH ** -0.5)
    conv_b_dw = nrm(ks[7], (P, D_MODEL), 0.02)
    conv_ln_g = 1.0 + nrm(ks[8], (P, D_MODEL), 0.05)
    conv_ln_b = nrm(ks[9], (P, D_MODEL), 0.02)
    conv_w_out = nrm(ks[10], (P, D_MODEL, D_MODEL), D_MODEL ** -0.5)
    pool_w = nrm(ks[11], (P, len(POOL_WINDOWS), POOL_GROUP, POOL_GROUP), POOL_GROUP ** -0.5)
    pool_scale = 1.0 + nrm(ks[12], (P, D_MODEL), 0.1)
    attn_w_in = nrm(ks[13], (P, D_MODEL, ATTN_IN_WIDTH), D_MODEL ** -0.5)
    attn_w_out = nrm(ks[14], (P, Q_WIDTH, D_MODEL), Q_WIDTH ** -0.5)
    n = jnp.arange(SSM_STATE, dtype=f32)
    ssm_lambda_re = -0.5 + nrm(ks[15], (P, SSM_GROUPS, SSM_STATE), 0.01)
    ssm_lambda_im = math.pi * n + nrm(ks[16], (P, SSM_GROUPS, SSM_STATE), 0.01)
    ssm_log_dt = jax.random.uniform(ks[17], (P, SSM_GROUPS), f32, math.log(1e-3), math.log(1e-1))
    ssm_b_re = nrm(ks[18], (P, SSM_GROUPS, SSM_STATE, SSM_GROUP), (2 * SSM_GROUP) ** -0.5)
    ssm_b_im = nrm(ks[19], (P, SSM_GROUPS, SSM_STATE, SSM_GROUP), (2 * SSM_GROUP) ** -0.5)
    ssm_c_re = nrm(ks[20], (P, SSM_GROUPS, SSM_GROUP, SSM_STATE), SSM_STATE ** -0.5)
    ssm_c_im = nrm(ks[21], (P, SSM_GROUPS, SSM_GROUP, SSM_STATE), SSM_STATE ** -0.5)
    ssm_d = nrm(ks[22], (P, D_MODEL), 1.0)
    ssm_w_glu = nrm(ks[23], (P, D_MODEL, 2 * D_MODEL), D_MODEL ** -0.5)
    return {'x': x, 'norm_gains': norm_gains, 'mlp_w_up': mlp_w_up, 'mlp_w_down': mlp_w_down,
            'conv_w_in': conv_w_in, 'conv_b_in': conv_b_in, 'conv_w_dw': conv_w_dw,
            'conv_b_dw': conv_b_dw, 'conv_ln_g': conv_ln_g, 'conv_ln_b': conv_ln_b,
            'conv_w_out': conv_w_out, 'pool_w': pool_w, 'pool_scale': pool_scale,
            'attn_w_in': attn_w_in, 'attn_w_out': attn_w_out,
            'ssm_lambda_re': ssm_lambda_re, 'ssm_lambda_im': ssm_lambda_im, 'ssm_log_dt': ssm_log_dt,
            'ssm_b_re': ssm_b_re, 'ssm_b_im': ssm_b_im, 'ssm_c_re': ssm_c_re, 'ssm_c_im': ssm_c_im,
            'ssm_d': ssm_d, 'ssm_w_glu': ssm_w_glu}


def reference(x, norm_gains, mlp_w_up, mlp_w_down, conv_w_in, conv_b_in, conv_w_dw, conv_b_dw,
              conv_ln_g, conv_ln_b, conv_w_out, pool_w, pool_scale, attn_w_in, attn_w_out,
              ssm_lambda_re, ssm_lambda_im, ssm_log_dt, ssm_b_re, ssm_b_im, ssm_c_re, ssm_c_im,
              ssm_d, ssm_w_glu):
    res = x
    for i in range(DEPTH):
        m = i % N_MIXERS
        j = i // N_MIXERS
        h = _rmsnorm(res, norm_gains[i, 0])
        if m == 0:
            h = _conv_module(h, conv_w_in[j], conv_b_in[j], conv_w_dw[j], conv_b_dw[j],
                             conv_ln_g[j], conv_ln_b[j], conv_w_out[j])
        elif m == 1:
            h = _pool_mixer(h, pool_w[j], pool_scale[j])
        elif m == 2:
            h = _sparse_attention(h, attn_w_in[j], attn_w_out[j])
        else:
            h = _s5_mixer(h, ssm_lambda_re[j], ssm_lambda_im[j], ssm_log_dt[j], ssm_b_re[j],
                          ssm_b_im[j], ssm_c_re[j], ssm_c_im[j], ssm_d[j], ssm_w_glu[j])
        res = res + _rmsnorm(h, norm_gains[i, 1])
        h = _rmsnorm(res, norm_gains[i, 2])
        h = _sq_relu_mlp(h, mlp_w_up[i], mlp_w_down[i])
        res = res + _rmsnorm(h, norm_gains[i, 3])
    return res
```

```python
import numpy as np
from contextlib import ExitStack
import concourse.bass as bass
import concourse.mybir as mybir
from concourse.bass_utils import run_bass_kernel_spmd

F32 = mybir.dt.float32
BF16 = mybir.dt.bfloat16
AF = mybir.ActivationFunctionType
ALU = mybir.AluOpType
AX = mybir.AxisListType

D = 2048
DC = 16
L = 8192
NCORE = 8
SEG = 512
NSEG = 16
DFF = 8192
EPS = 1e-6
LN_EPS = 1e-5
HALO = 32


class _Op:
    __slots__ = ("eng", "fn", "deps", "inc", "val", "lane")

    def __init__(self, eng, fn, lane=None):
        self.eng = eng
        self.fn = fn
        self.deps = []
        self.inc = False
        self.val = 0
        self.lane = lane


class Sched:
    ENGS = ("pe", "act", "dve", "pool", "sp")

    def __init__(self, nc):
        self.nc = nc
        self.ops = {e: [] for e in self.ENGS}
        self.res = {}
        self.lanes = {}
        self.batch_lanes = set()

    def _dep(self, op, reads, writes):
        deps = []
        for k in reads:
            ent = self.res.get(k)
            if ent is not None and ent[0] is not None:
                deps.append(ent[0])
        for k in writes:
            ent = self.res.get(k)
            if ent is not None:
                if ent[0] is not None:
                    deps.append(ent[0])
                deps.extend(ent[1])
        for k in reads:
            ent = self.res.setdefault(k, [None, []])
            ent[1].append(op)
        for k in writes:
            self.res[k] = [op, []]
        seen = set()
        for d in deps:
            if d is op or id(d) in seen:
                continue
            seen.add(id(d))
            if d.lane is None and d.eng == op.eng and op.eng == "pe":
                continue
            if d.lane is not None and d.lane == op.lane and d.lane in self.batch_lanes:
                continue
            op.deps.append(d)

    def op(self, eng, fn, reads=(), writes=()):
        o = _Op(eng, fn)
        self.ops[eng].append(o)
        self._dep(o, reads, writes)
        return o

    def dma(self, eng, lane, fn, reads=(), writes=()):
        o = _Op(eng, fn, lane=lane)
        self.ops[eng].append(o)
        self.lanes.setdefault(lane, []).append(o)
        o.val = 16 * len(self.lanes[lane])
        self._dep(o, reads, writes)
        return o

    def emit(self, final_waits=()):
        nc = self.nc
        for ln in self.batch_lanes:
            tot = 16 * len(self.lanes[ln])
            for o in self.lanes[ln]:
                o.val = tot
        for e in self.ENGS:
            for o in self.ops[e]:
                for d in o.deps:
                    d.inc = True
        for e in self.ENGS:
            c = 0
            for o in self.ops[e]:
                if o.lane is None and o.inc:
                    c += 1
                    o.val = c
        with ExitStack() as st:
            sems = {}
            for e in self.ENGS:
                sems[e] = st.enter_context(nc.semaphore("s_" + e))
            for ln in self.lanes:
                sems[("lane", ln)] = st.enter_context(nc.semaphore("l_" + str(ln)))
            block = st.enter_context(nc.Block())
            hmap = {"pe": "tensor", "act": "scalar", "dve": "vector", "pool": "gpsimd", "sp": "sync"}

            def semof(o):
                return sems[("lane", o.lane)] if o.lane is not None else sems[o.eng]

            def body(e):
                def run(engh):
                    waited = {}
                    for o in self.ops[e]:
                        for d in o.deps:
                            s = semof(d)
                            key = id(s)
                            if waited.get(key, 0) >= d.val:
                                continue
                            waited[key] = d.val
                            engh.wait_ge(s, d.val)
                        ins = o.fn()
                        if o.lane is not None:
                            ins.then_inc(sems[("lane", o.lane)], 16)
                        elif o.inc:
                            ins.then_inc(sems[e], 1)
                    if e == "sp":
                        for o in final_waits:
                            engh.wait_ge(semof(o), o.val)
                return run

            for e in self.ENGS:
                getattr(block, hmap[e])(body(e))


class Prog:
    NSLOT = 2

    def __init__(self):
        self.nc = bass.Bass("TRN2", target_bir_lowering=False)
        self.st = ExitStack()
        self.S = Sched(self.nc)
        self.nbank = 0
        self.nslot = 0
        self.uid = 0
        nc = self.nc
        self.banks = [self.st.enter_context(nc.psum_tensor(f"ps{i}", [128, 512], F32)) for i in range(8)]
        self.slots = [self.st.enter_context(nc.sbuf_tensor(f"wslot{i}", [128, 16, 512], BF16))
                      for i in range(self.NSLOT)]
        self.ones = self.sb("ones", [128, 128], BF16)
        self.S.op("dve", lambda: nc.vector.memset(self.ones[:], 1.0), writes=["ones"])
        self.outs = []

    def sb(self, name, shape, dt):
        return self.st.enter_context(self.nc.sbuf_tensor("sb_" + name, shape, dt))

    def dram_in(self, name, shape, dt=F32):
        return self.nc.dram_tensor(name, list(shape), dt, kind="ExternalInput").ap()

    def dram_out(self, name, shape, dt=F32):
        return self.nc.dram_tensor(name, list(shape), dt, kind="ExternalOutput").ap()

    bank_list = list(range(8))

    def bank(self):
        b = self.bank_list[self.nbank % len(self.bank_list)]
        self.nbank += 1
        return b

    def load(self, dst_ap, src_ap, key, eng="sp", lane=None):
        nc = self.nc
        if lane is None:
            self.uid += 1
            lane = f"ld{self.uid}"
        else:
            self.S.batch_lanes.add(lane)
        h = nc.sync if eng == "sp" else nc.gpsimd
        return self.S.dma(eng, lane, lambda: h.dma_start(out=dst_ap, in_=src_ap), writes=[key])

    def store(self, dst_ap, src_ap, keys, lane=None, batch=True, eng="sp"):
        nc = self.nc
        if lane is None:
            self.uid += 1
            lane = f"st{self.uid}"
        elif batch:
            self.S.batch_lanes.add(lane)
        h = nc.sync if eng == "sp" else nc.gpsimd
        o = self.S.dma(eng, lane, lambda: h.dma_start(out=dst_ap, in_=src_ap), reads=keys)
        self.outs.append(o)
        return o

    def finish(self):
        self.S.emit(final_waits=self.outs)
        self.st.close()
        return self.nc

    def load_slot(self, pieces, pre=None):
        nc = self.nc
        s = self.nslot % self.NSLOT
        self.nslot += 1
        slot = self.slots[s]
        if pre is not None:
            dst = slot[:, :, :].rearrange("p k m -> p (k m)")
            self.S.dma("pool", f"slot{s}",
                       (lambda dst=dst, src=pre: nc.gpsimd.dma_start(out=dst, in_=src, max_dma_last_dim=8192)),
                       writes=[("slot", s, p[4]) for p in pieces])
            return s
        for (W, k0, m0, ncols, off) in pieces:
            src = W[k0:k0 + 2048, m0:m0 + ncols].rearrange("(kc p) m -> p kc m", p=128)
            dst = slot[:, :, off:off + ncols]
            self.S.dma("pool", f"slot{s}",
                       (lambda dst=dst, src=src: nc.gpsimd.dma_start(out=dst, in_=src, max_dma_last_dim=4096)),
                       writes=[("slot", s, off)])
        return s

    def linear(self, W, K, M, rhs_fn, rhs_keys_fn, N, evac, col_groups=None, blocks_fn=None, pre=False):
        nc = self.nc
        if col_groups is None:
            col_groups = [[(m0, 512)] for m0 in range(0, M, 512)]
        nk = K // 2048
        for gi, grp in enumerate(col_groups):
            blocks = blocks_fn(gi) if blocks_fn is not None else [(0, 128), (128, 128), (256, 128), (384, 128)]
            nb = len(blocks)
            bks = [self.bank() for _ in range(nb)]
            for ks in range(nk):
                pieces = []
                off = 0
                for (m0, ncols) in grp:
                    pieces.append((W, ks * 2048, m0, ncols, off))
                    off += ncols
                s = self.load_slot(pieces, pre=(W[gi * nk + ks] if pre else None))
                slot = self.slots[s]
                rkeys = [("slot", s, p[4]) for p in pieces]
                for mb in range(nb):
                    o0, wd = blocks[mb]
                    for kc in range(16):
                        kk = ks * 16 + kc
                        first = (ks == 0 and kc == 0)
                        last = (ks == nk - 1 and kc == 15)
                        self.S.op("pe",
                                  (lambda b=bks[mb], slot=slot, kc=kc, o0=o0, wd=wd, kk=kk, first=first, last=last:
                                   nc.tensor.matmul(self.banks[b][:wd, :N], lhsT=slot[:, kc, o0:o0 + wd],
                                                    rhs=rhs_fn(kk), start=first, stop=last)),
                                  reads=rkeys + list(rhs_keys_fn(kk)), writes=[("ps", bks[mb])])
            evac(gi, bks)

    def rms_rstd(self, src, skey, ncol0, N, scratch, sckey, rstd, rkey, nch=DC):
        nc = self.nc
        b = self.bank()
        for c in range(nch):
            self.S.op("act", (lambda c=c: nc.scalar.activation(out=scratch[:, c, :N], in_=src[:, c, ncol0:ncol0 + N],
                                                               func=AF.Square)),
                      reads=[(skey, c)], writes=[(sckey, c)])
            self.S.op("pe", (lambda c=c: nc.tensor.matmul(self.banks[b][:, :N], lhsT=self.ones[:], rhs=scratch[:, c, :N],
                                                          start=(c == 0), stop=(c == nch - 1))),
                      reads=[(sckey, c), "ones"], writes=[("ps", b)])
        self.S.op("act", lambda: nc.scalar.activation(out=rstd[:, :N], in_=self.banks[b][:, :N], func=AF.Sqrt,
                                                      scale=1.0 / (128 * nch), bias=self.epsc[:, 0:1]),
                  reads=[("ps", b), "epsc"], writes=[rkey])
        self.S.op("dve", lambda: nc.vector.reciprocal(out=rstd[:, :N], in_=rstd[:, :N]), reads=[rkey], writes=[rkey])

    def consts(self):
        nc = self.nc
        self.epsc = self.sb("epsc", [128, 2], F32)
        self.S.op("dve", lambda: nc.vector.memset(self.epsc[:, 0:1], EPS), writes=["epsc"])
        self.S.op("dve", lambda: nc.vector.memset(self.epsc[:, 1:2], LN_EPS), writes=["epsc"])

    def norm_apply(self, src, skey, ncol0, N, gains, gcol0, rstd, rkey, dst, dkey, dcol0, nch=DC, eng="dve"):
        nc = self.nc
        for c in range(nch):
            self.S.op("dve", (lambda c=c: nc.vector.scalar_tensor_tensor(
                out=dst[:, c, dcol0:dcol0 + N], in0=src[:, c, ncol0:ncol0 + N],
                scalar=gains[:, gcol0 + c:gcol0 + c + 1], in1=rstd[:, :N], op0=ALU.mult, op1=ALU.mult)),
                reads=[(skey, c), rkey, "gains"], writes=[(dkey, c)])

    def norm_add(self, src, skey, N, gains, gcol0, rstd, rkey, res, reskey, rcol0, tmp, tkey):
        nc = self.nc
        for c in range(DC):
            self.S.op("dve", (lambda c=c: nc.vector.scalar_tensor_tensor(
                out=tmp[:, c % 2, :N], in0=src[:, c, :N], scalar=gains[:, gcol0 + c:gcol0 + c + 1],
                in1=rstd[:, :N], op0=ALU.mult, op1=ALU.mult)),
                reads=[(skey, c), rkey, "gains"], writes=[(tkey, c % 2)])
            self.S.op("dve", (lambda c=c: nc.vector.tensor_tensor(
                out=res[:, c, rcol0:rcol0 + N], in0=res[:, c, rcol0:rcol0 + N], in1=tmp[:, c % 2, :N], op=ALU.add)),
                reads=[(tkey, c % 2), (reskey, c)], writes=[(reskey, c)])

    def mlp(self, res, reskey, rcol0, N, gains, g_in, g_out, w_up, w_down, xn, xnkey, hbuf, tmpA, rstd, tmp2):
        nc = self.nc
        S = self.S
        self.rms_rstd(res, reskey, rcol0, N, hbuf, "h", rstd, "rstd")
        self.norm_apply(res, reskey, rcol0, N, gains, g_in, rstd, "rstd", xn, xnkey, 0)

        def evac_up(gi, bks):
            for mb in range(4):
                f = gi * 4 + mb
                S.op("act", (lambda b=bks[mb], mb=mb: nc.scalar.activation(out=tmp2[:, mb, :N], in_=self.banks[b][:, :N],
                                                                          func=AF.Relu)),
                     reads=[("ps", bks[mb])], writes=[("tmp2", mb)])
                S.op("dve", (lambda b=bks[mb], mb=mb, f=f: nc.vector.tensor_tensor(
                    out=hbuf[:, f, :N], in0=self.banks[b][:, :N], in1=tmp2[:, mb, :N], op=ALU.mult)),
                    reads=[("ps", bks[mb]), ("tmp2", mb)], writes=[("h", f)])

        self.linear(w_up, D, DFF, lambda kk: xn[:, kk, :N], lambda kk: [(xnkey, kk)], N, evac_up, pre=True)

        def evac_down(gi, bks):
            for mb in range(4):
                c = gi * 4 + mb
                S.op("act", (lambda b=bks[mb], c=c: nc.scalar.copy(out=tmpA[:, c, :N], in_=self.banks[b][:, :N])),
                     reads=[("ps", bks[mb])], writes=[("tmpA", c)])

        self.linear(w_down, DFF, D, lambda kk: hbuf[:, kk, :N], lambda kk: [("h", kk)], N, evac_down, pre=True)
        self.rms_rstd(tmpA, "tmpA", 0, N, xn, xnkey, rstd, "rstd")
        self.norm_add(tmpA, "tmpA", N, gains, g_out, rstd, "rstd", res, reskey, rcol0, tmp2, "tmp2")


def run(prog_nc, in_maps):
    return run_bass_kernel_spmd(prog_nc, in_maps, core_ids=list(range(NCORE))).results


def seg_of(core):
    return (core, NSEG - 1 - core)


def to_fm(a):
    T, F = a.shape
    return np.ascontiguousarray(a.T.reshape(F // 128, 128, T))


def from_fm(a):
    C, P, T = a.shape
    return np.ascontiguousarray(a.reshape(C * P, T).T)


def col_layout(v):
    return np.ascontiguousarray(v.reshape(-1, 128).T)


def build_layer0():
    P = Prog()
    nc, S = P.nc, P.S
    N = SEG
    NH = N + HALO
    xin_d = P.dram_in("xin", [2, DC, 128, N])
    xh_d = P.dram_in("xh", [DC, 128, 2 * HALO])
    hmask_d = P.dram_in("hmask", [128, 2 * HALO])
    gains_d = P.dram_in("gains", [128, 4 * DC])
    vecs_d = P.dram_in("vecs", [128, 6 * DC])
    wdw_d = P.dram_in("wdw", [128, DC, 31])
    ident_d = P.dram_in("ident", [128, 128])
    w_in = P.dram_in("w_in", [8, 128, 8192])
    w_out = P.dram_in("w_out", [4, 128, 8192])
    w_up = P.dram_in("w_up", [16, 128, 8192])
    w_down = P.dram_in("w_down", [16, 128, 8192])
    out_d = P.dram_out("out", [2, DC, 128, N])

    P.consts()
    xin = P.sb("xin", [128, DC, N], F32)
    xh = P.sb("xh", [128, DC, 2 * HALO], F32)
    xn = P.sb("xn", [128, DC, N], BF16)
    tmpA = P.sb("tmpA", [128, DC, N], F32)
    big = P.sb("big", [128, 64 * N], BF16)
    hbuf = big[:, :].rearrange("p (c n) -> p c n", n=N)
    hglu = big[:, 32 * N:32 * N + DC * NH].rearrange("p (c n) -> p c n", n=NH)
    diag = big[:, 50 * N:50 * N + 31 * 128].rearrange("p (k m) -> p k m", m=128)

    def hglu_alias(c):
        lo = (32 * N + c * NH) // N
        hi = (32 * N + (c + 1) * NH - 1) // N
        return [("h", f) for f in range(lo, hi + 1)]
    diag_alias = [("h", f) for f in range(50, 58)]
    rstd = P.sb("rstd", [128, N], F32)
    mean = P.sb("mean", [128, N], F32)
    tmp2 = P.sb("tmp2", [128, 4, N], F32)
    gains = P.sb("gains_sb", [128, 4 * DC], F32)
    vecs = P.sb("vecs_sb", [128, 6 * DC], F32)
    wdw = P.sb("wdw_sb", [128, DC, 31], F32)
    ident = P.sb("ident_sb", [128, 128], F32)
    hmask = P.sb("hmask_sb", [128, 2 * HALO], F32)
    hgh = P.sb("hgh", [128, DC, 2 * HALO], BF16)

    P.load(gains[:], gains_d[:, :], "gains")
    P.load(vecs[:], vecs_d[:, :], "vecs")
    P.load(wdw[:], wdw_d[:, :, :], "wdw")
    P.load(ident[:], ident_d[:, :], "ident")
    P.load(hmask[:], hmask_d[:, :], "hmask")
    for c in range(DC):
        P.load(xh[:, c, :], xh_d[c], ("xh", c), lane="xh")

    groups = [[(256 * g, 256), (2048 + 256 * g, 256)] for g in range(8)]

    def inproj(src, skey, n, dst, dkey, dcol0, masked):
        P.rms_rstd(src, skey, 0, n, hbuf, "h", rstd, "rstd")
        P.norm_apply(src, skey, 0, n, gains, 0, rstd, "rstd", xn, "xn", 0)

        def evac_in(gi, bks):
            for j in range(2):
                c = gi * 2 + j
                ba, bg = bks[j], bks[2 + j]
                S.op("act", (lambda bg=bg, c=c, j=j: nc.scalar.activation(
                    out=tmp2[:, j, :n], in_=P.banks[bg][:, :n], func=AF.Sigmoid,
                    bias=vecs[:, DC + c:DC + c + 1])),
                    reads=[("ps", bg), "vecs"], writes=[("tmp2", j)])
                S.op("dve", (lambda ba=ba, c=c, j=j: nc.vector.scalar_tensor_tensor(
                    out=dst[:, c, dcol0:dcol0 + n], in0=P.banks[ba][:, :n], scalar=vecs[:, c:c + 1],
                    in1=tmp2[:, j, :n], op0=ALU.add, op1=ALU.mult)),
                    reads=[("ps", ba), ("tmp2", j), "vecs"], writes=[(dkey, c)] + (hglu_alias(c) if dkey == "hglu" else []))
                if masked:
                    S.op("dve", (lambda c=c: nc.vector.tensor_tensor(
                        out=dst[:, c, 0:n], in0=dst[:, c, 0:n], in1=hmask[:], op=ALU.mult)),
                        reads=[(dkey, c), "hmask"], writes=[(dkey, c)])
        P.linear(w_in, D, 2 * D, (lambda kk: xn[:, kk, :n]), lambda kk: [("xn", kk)], n,
                 evac_in, col_groups=groups, pre=True)

    inproj(xh, "xh", 2 * HALO, hgh, "hgh", 0, True)

    for t in range(2):
        for c in range(DC):
            P.load(xin[:, c, :], xin_d[t, c], ("xin", c), lane=f"xin{t}")
        for c in range(DC):
            S.op("dve", (lambda c=c, t=t: nc.vector.tensor_copy(out=hglu[:, c, 0:HALO],
                                                                 in_=hgh[:, c, t * HALO:(t + 1) * HALO])),
                 reads=[("hgh", c)], writes=[("hglu", c)] + hglu_alias(c))
        inproj(xin, "xin", N, hglu, "hglu", HALO, False)
        for c in range(DC):
            for k in range(31):
                S.op("dve", (lambda c=c, k=k: nc.vector.tensor_scalar(
                    out=diag[:, k, :], in0=ident[:], scalar1=wdw[:, c, k:k + 1], scalar2=None, op0=ALU.mult)),
                    reads=["ident", "wdw"], writes=[("diag", k)] + (diag_alias if k == 0 else []))
            b = P.bank()
            for k in range(31):
                S.op("pe", (lambda c=c, k=k, b=b: nc.tensor.matmul(
                    P.banks[b][:, :N], lhsT=diag[:, k, :], rhs=hglu[:, c, 2 + k:2 + k + N],
                    start=(k == 0), stop=(k == 30))),
                    reads=[("diag", k), ("hglu", c)] + hglu_alias(c) + diag_alias, writes=[("ps", b)])
            S.op("act", (lambda c=c, b=b: nc.scalar.activation(
                out=tmpA[:, c, :], in_=P.banks[b][:, :N], func=AF.Identity, bias=vecs[:, 2 * DC + c:2 * DC + c + 1])),
                reads=[("ps", b), "vecs"], writes=[("tmpA", c)])
        bs, bq = P.bank(), P.bank()
        for c in range(DC):
            S.op("act", (lambda c=c: nc.scalar.copy(out=hbuf[:, c, :N], in_=tmpA[:, c, :])),
                 reads=[("tmpA", c)], writes=[("h", c)])
            S.op("act", (lambda c=c: nc.scalar.activation(out=hbuf[:, 16 + c, :N], in_=tmpA[:, c, :], func=AF.Square)),
                 reads=[("tmpA", c)], writes=[("h", 16 + c)])
            S.op("pe", (lambda c=c, bs=bs: nc.tensor.matmul(P.banks[bs][:, :N], lhsT=P.ones[:], rhs=hbuf[:, c, :N],
                                                     start=(c == 0), stop=(c == DC - 1))),
                 reads=[("h", c), "ones"], writes=[("ps", bs)])
            S.op("pe", (lambda c=c, bq=bq: nc.tensor.matmul(P.banks[bq][:, :N], lhsT=P.ones[:], rhs=hbuf[:, 16 + c, :N],
                                                     start=(c == 0), stop=(c == DC - 1))),
                 reads=[("h", 16 + c), "ones"], writes=[("ps", bq)])
        S.op("act", lambda bs=bs: nc.scalar.activation(out=mean[:], in_=P.banks[bs][:, :N], func=AF.Copy, scale=1.0 / D),
             reads=[("ps", bs)], writes=["mean"])
        S.op("dve", lambda: nc.vector.tensor_tensor(out=tmp2[:, 0, :], in0=mean[:], in1=mean[:], op=ALU.mult),
             reads=["mean"], writes=[("tmp2", 0)])
        S.op("dve", lambda bq=bq: nc.vector.scalar_tensor_tensor(out=tmp2[:, 1, :], in0=P.banks[bq][:, :N], scalar=1.0 / D,
                                                           in1=tmp2[:, 0, :], op0=ALU.mult, op1=ALU.subtract),
             reads=[("ps", bq), ("tmp2", 0)], writes=[("tmp2", 1)])
        S.op("act", lambda: nc.scalar.activation(out=rstd[:, :N], in_=tmp2[:, 1, :], func=AF.Sqrt,
                                                 bias=P.epsc[:, 1:2]),
             reads=[("tmp2", 1), "epsc"], writes=["rstd"])
        S.op("dve", lambda: nc.vector.reciprocal(out=rstd[:, :N], in_=rstd[:, :N]), reads=["rstd"], writes=["rstd"])
        for c in range(DC):
            j = 2 + (c % 2)
            S.op("dve", (lambda c=c, j=j: nc.vector.tensor_tensor(out=tmp2[:, j, :], in0=tmpA[:, c, :], in1=mean[:],
                                                                  op=ALU.subtract)),
                 reads=[("tmpA", c), "mean"], writes=[("tmp2", j)])
            S.op("dve", (lambda c=c, j=j: nc.vector.tensor_tensor(out=tmp2[:, j, :], in0=tmp2[:, j, :], in1=rstd[:, :N],
                                                                  op=ALU.mult)),
                 reads=[("tmp2", j), "rstd"], writes=[("tmp2", j)])
            S.op("act", (lambda c=c, j=j: nc.scalar.activation(
                out=xn[:, c, :N], in_=tmp2[:, j, :], func=AF.Silu,
                scale=vecs[:, 3 * DC + c:3 * DC + c + 1], bias=vecs[:, 4 * DC + c:4 * DC + c + 1])),
                reads=[("tmp2", j), "vecs"], writes=[("xn", c)])

        def evac_out(gi, bks):
            for mb in range(4):
                c = gi * 4 + mb
                S.op("act", (lambda b=bks[mb], c=c: nc.scalar.copy(out=tmpA[:, c, :], in_=P.banks[b][:, :N])),
                     reads=[("ps", bks[mb])], writes=[("tmpA", c)])
        P.linear(w_out, D, D, lambda kk: xn[:, kk, :N], lambda kk: [("xn", kk)], N, evac_out, pre=True)
        P.rms_rstd(tmpA, "tmpA", 0, N, hbuf, "h", rstd, "rstd")
        P.norm_add(tmpA, "tmpA", N, gains, DC, rstd, "rstd", xin, "xin", 0, tmp2, "tmp2")
        P.mlp(xin, "xin", 0, N, gains, 2 * DC, 3 * DC, w_up, w_down, xn, "xn", hbuf, tmpA, rstd, tmp2)
        for c in range(DC):
            P.store(out_d[t, c], xin[:, c, :], [("xin", c)], lane=f"out{t}")
    return P.finish()


POOL_W = (2, 4, 8, 16)


def build_layer1():
    P = Prog()
    nc, S = P.nc, P.S
    N = SEG
    HALO = 16
    NH = N + HALO
    xin_d = P.dram_in("xin", [2, DC, 128, N])
    xh_d = P.dram_in("xh", [DC, 128, 2 * HALO])
    rdiv_d = P.dram_in("rdiv", [2, 128, 4, N])
    gains_d = P.dram_in("gains", [128, 4 * DC])
    pscale_d = P.dram_in("pscale", [128, DC])
    w_pool = P.dram_in("w_pool", [D, 512])
    w_up = P.dram_in("w_up", [16, 128, 8192])
    w_down = P.dram_in("w_down", [16, 128, 8192])
    out_d = P.dram_out("out", [2, DC, 128, N])

    P.consts()
    xin = P.sb("xin", [128, DC, N], F32)
    xh = P.sb("xh", [128, DC, 2 * HALO], F32)
    hh = P.sb("hh", [128, DC, 2 * HALO], F32)
    xn = P.sb("xn", [128, DC, N], BF16)
    tmpA = P.sb("tmpA", [128, DC, NH], F32)
    hbuf = P.sb("hbuf", [128, 64, N], BF16)
    rstd = P.sb("rstd", [128, N], F32)
    tmp2 = P.sb("tmp2", [128, 4, N], F32)
    sbuf2 = P.sb("sbuf2", [128, 2, NH], F32)
    rdiv = P.sb("rdiv", [128, 4, N], F32)
    gains = P.sb("gains_sb", [128, 4 * DC], F32)
    pscale = P.sb("pscale_sb", [128, DC], F32)

    P.load(gains[:], gains_d[:, :], "gains")
    P.load(pscale[:], pscale_d[:, :], "pscale")
    for c in range(DC):
        P.load(xh[:, c, :], xh_d[c], ("xh", c), lane="xh")
    P.rms_rstd(xh, "xh", 0, 2 * HALO, hbuf, "h", rstd, "rstd")
    for c in range(DC):
        S.op("dve", (lambda c=c: nc.vector.scalar_tensor_tensor(
            out=hh[:, c, :], in0=xh[:, c, :], scalar=gains[:, c:c + 1], in1=rstd[:, :2 * HALO],
            op0=ALU.mult, op1=ALU.mult)), reads=[("xh", c), "rstd", "gains"], writes=[("hh", c)])

    for t in range(2):
        P.load(rdiv[:], rdiv_d[t], "rdiv")
        for c in range(DC):
            P.load(xin[:, c, :], xin_d[t, c], ("xin", c), lane=f"xin{t}")
        P.rms_rstd(xin, "xin", 0, N, hbuf, "h", rstd, "rstd")
        hf = tmpA
        for c in range(DC):
            S.op("dve", (lambda c=c, t=t: nc.vector.tensor_copy(out=hf[:, c, 0:HALO], in_=hh[:, c, t * HALO:(t + 1) * HALO])),
                 reads=[("hh", c)], writes=[("tmpA", c)])
            S.op("dve", (lambda c=c: nc.vector.scalar_tensor_tensor(
                out=hf[:, c, HALO:NH], in0=xin[:, c, :], scalar=gains[:, c:c + 1], in1=rstd[:, :N],
                op0=ALU.mult, op1=ALU.mult)), reads=[("xin", c), "rstd", "gains", ("tmpA", c)], writes=[("tmpA", c)])
        for c in range(DC):
            g = c // 4
            nstep = g + 1
            cur = hf[:, c, :]
            curkey = ("tmpA", c)
            for si in range(nstep):
                sh = 1 << si
                lo = 2 * sh - 1
                dst = sbuf2[:, si % 2, :]
                dkey = ("sbuf2", si % 2)
                S.op("dve", (lambda cur=cur, dst=dst, sh=sh, lo=lo: nc.vector.tensor_tensor(
                    out=dst[:, lo:NH], in0=cur[:, lo:NH], in1=cur[:, lo - sh:NH - sh], op=ALU.add)),
                    reads=[curkey], writes=[dkey])
                cur, curkey = dst, dkey
            S.op("dve", (lambda cur=cur, g=g: nc.vector.tensor_tensor(
                out=tmp2[:, 0, :], in0=cur[:, HALO:NH], in1=rdiv[:, g, :], op=ALU.mult)),
                reads=[curkey, "rdiv"], writes=[("tmp2", 0)])
            S.op("dve", (lambda c=c: nc.vector.tensor_tensor(
                out=xn[:, c, :], in0=tmp2[:, 0, :], in1=hf[:, c, HALO:NH], op=ALU.subtract)),
                reads=[("tmp2", 0), ("tmpA", c)], writes=[("xn", c)])
        s = P.load_slot([(w_pool, 0, 0, 512, 0)])
        slot = P.slots[s]
        for g in range(4):
            bks = [P.bank() for _ in range(4)]
            for mb in range(4):
                for kc in range(4):
                    S.op("pe", (lambda g=g, mb=mb, kc=kc, b=bks[mb], slot=slot: nc.tensor.matmul(
                        P.banks[b][:, :N], lhsT=slot[:, g * 4 + kc, mb * 128:(mb + 1) * 128], rhs=xn[:, g * 4 + kc, :],
                        start=(kc == 0), stop=(kc == 3))),
                        reads=[("slot", s, 0), ("xn", g * 4 + kc)], writes=[("ps", bks[mb])])
            for mb in range(4):
                c = g * 4 + mb
                S.op("act", (lambda b=bks[mb], c=c: nc.scalar.activation(
                    out=tmpA[:, c, :N], in_=P.banks[b][:, :N], func=AF.Copy, scale=pscale[:, c:c + 1])),
                    reads=[("ps", bks[mb]), "pscale"], writes=[("tmpA", c)])
        P.rms_rstd(tmpA, "tmpA", 0, N, hbuf, "h", rstd, "rstd")
        P.norm_add(tmpA, "tmpA", N, gains, DC, rstd, "rstd", xin, "xin", 0, tmp2, "tmp2")
        P.mlp(xin, "xin", 0, N, gains, 2 * DC, 3 * DC, w_up, w_down, xn, "xn", hbuf, tmpA, rstd, tmp2)
        for c in range(DC):
            P.store(out_d[t, c], xin[:, c, :], [("xin", c)], lane=f"out{t}")
    return P.finish()


QW, KVW, QIW, IDXD, IDXH = 2048, 512, 1024, 64, 16
ROPE_THETA = 500000.0


def build_l2a():
    P = Prog()
    nc, S = P.nc, P.S
    N = SEG
    xin_d = P.dram_in("xin", [2, DC, 128, N])
    rope_d = P.dram_in("rope", [2, 128, 4, N])
    rot_d = P.dram_in("rot", [128, 2, 128])
    gains_d = P.dram_in("gains", [128, 4 * DC])
    w_in = P.dram_in("w_in", [D, 4176])
    q_d = P.dram_out("q", [2, 16, 128, N], BF16)
    k_d = P.dram_out("k", [2, 4, 128, N], BF16)
    v_d = P.dram_out("v", [2, 4, 128, N], BF16)
    qi_d = P.dram_out("qi", [2, 8, 128, N], BF16)
    ki_d = P.dram_out("ki", [2, 64, N], BF16)
    wi_d = P.dram_out("wi", [2, 16, N], F32)

    P.consts()
    xin = P.sb("xin", [128, DC, N], F32)
    xn = P.sb("xn", [128, DC, N], BF16)
    scr = P.sb("scr", [128, DC, N], BF16)
    rstd = P.sb("rstd", [128, N], F32)
    outb = P.sb("outb", [128, 34, N], BF16)
    wib = P.sb("wib", [128, N], F32)
    qraw = P.sb("qraw", [128, 8, N], BF16)
    tt = P.sb("tt", [128, 4, N], F32)
    rope = P.sb("rope_sb", [128, 4, N], F32)
    rotf = P.sb("rotf", [128, 2, 128], F32)
    rot = P.sb("rot_sb", [128, 2, 128], BF16)
    gains = P.sb("gains_sb", [128, 4 * DC], F32)
    P.load(gains[:], gains_d[:, :], "gains")
    P.load(rotf[:], rot_d[:, :, :], "rotf")
    S.op("dve", lambda: nc.vector.tensor_copy(out=rot[:], in_=rotf[:]), reads=["rotf"], writes=["rot"])

    groups = [[(512 * g, 512)] for g in range(8)] + [[(4096, 80)]]

    def blocks_fn(gi):
        return [(0, 80)] if gi == 8 else [(0, 128), (128, 128), (256, 128), (384, 128)]

    for t in range(2):
        P.load(rope[:], rope_d[t], "rope")
        for c in range(DC):
            P.load(xin[:, c, :], xin_d[t, c], ("xin", c), lane=f"xin{t}")
        P.rms_rstd(xin, "xin", 0, N, scr, "scr", rstd, "rstd")
        P.norm_apply(xin, "xin", 0, N, gains, 0, rstd, "rstd", xn, "xn", 0)
        deferred = []
        cnt = [0]

        def rope_chunk(b, oc, np_, ti, t=t):
            j = cnt[0] % 8
            cnt[0] += 1
            S.op("act", (lambda: nc.scalar.copy(out=qraw[:np_, j, :], in_=P.banks[b][:np_, :N])),
                 reads=[("ps", b)], writes=[("qraw", j)])

            def later():
                rb = P.bank()
                S.op("pe", (lambda: nc.tensor.matmul(P.banks[rb][:np_, :N], lhsT=rot[:np_, ti, :np_], rhs=qraw[:np_, j, :],
                                                     start=True, stop=True)),
                     reads=[("qraw", j), "rot"], writes=[("ps", rb)])
                a = (2 * j) % 4
                S.op("dve", (lambda: nc.vector.tensor_tensor(out=tt[:np_, a, :], in0=qraw[:np_, j, :],
                                                             in1=rope[:np_, 2 * ti, :], op=ALU.mult)),
                     reads=[("qraw", j), "rope"], writes=[("tt", a)])
                S.op("dve", (lambda: nc.vector.tensor_tensor(out=tt[:np_, a + 1, :], in0=P.banks[rb][:np_, :N],
                                                             in1=rope[:np_, 2 * ti + 1, :], op=ALU.mult)),
                     reads=[("ps", rb), "rope"], writes=[("tt", a + 1)])
                S.op("dve", (lambda: nc.vector.tensor_tensor(out=outb[:np_, oc, :], in0=tt[:np_, a, :],
                                                             in1=tt[:np_, a + 1, :], op=ALU.add)),
                     reads=[("tt", a), ("tt", a + 1)], writes=[("outb", oc)])
            deferred.append(later)

        def evac(gi, bks, t=t):
            prev = list(deferred)
            del deferred[:]
            if gi < 5:
                for mb in range(4):
                    rope_chunk(bks[mb], gi * 4 + mb, 128, 0)
            elif gi == 5:
                for mb in range(4):
                    S.op("act", (lambda b=bks[mb], oc=20 + mb: nc.scalar.copy(out=outb[:, oc, :], in_=P.banks[b][:, :N])),
                         reads=[("ps", bks[mb])], writes=[("outb", 20 + mb)])
            elif gi < 8:
                for mb in range(4):
                    rope_chunk(bks[mb], 24 + (gi - 6) * 4 + mb, 128, 1)
            else:
                b = bks[0]
                S.op("act", (lambda b=b: nc.scalar.activation(out=wib[64:80, :], in_=P.banks[b][64:80, :N], func=AF.Copy,
                                                              scale=1.0 / 32.0)),
                     reads=[("ps", b)], writes=["wib"])
                rope_chunk(b, 32, 64, 1)
            for f in prev:
                f()

        P.linear(w_in, D, 4176, lambda kk: xn[:, kk, :N], lambda kk: [("xn", kk)], N, evac,
                 col_groups=groups, blocks_fn=blocks_fn)
        for f in deferred:
            f()
        del deferred[:]
        for c in range(16):
            P.store(q_d[t, c], outb[:, c, :], [("outb", c)], lane=f"out{t}")
        for c in range(4):
            P.store(k_d[t, c], outb[:, 16 + c, :], [("outb", 16 + c)], lane=f"out{t}")
            P.store(v_d[t, c], outb[:, 20 + c, :], [("outb", 20 + c)], lane=f"out{t}")
        for c in range(8):
            P.store(qi_d[t, c], outb[:, 24 + c, :], [("outb", 24 + c)], lane=f"out{t}")
        P.store(ki_d[t], outb[0:64, 32, :], [("outb", 32)], lane=f"out{t}")
        P.store(wi_d[t], wib[64:80, :], ["wib"], lane=f"out{t}")
    return P.finish()


def rope_tables(seg):
    pos = np.arange(seg * SEG, (seg + 1) * SEG, dtype=np.float64)
    tab = np.zeros((128, 4, SEG), np.float32)
    tab[:, 0, :] = 1.0
    tab[:, 2, :] = 1.0
    invq = ROPE_THETA ** (-2.0 * np.arange(16) / 32.0)
    invi = ROPE_THETA ** (-2.0 * np.arange(8) / 16.0)
    angq = (pos.astype(np.float32)[None, :] * invq.astype(np.float32)[:, None]).astype(np.float32)
    angi = (pos.astype(np.float32)[None, :] * invi.astype(np.float32)[:, None]).astype(np.float32)
    for m in range(32):
        tab[m, 0, :] = np.cos(angq[m % 16])
        tab[m, 1, :] = np.sin(angq[m % 16])
    for b in (0, 64):
        for jj in range(16):
            tab[b + jj, 2, :] = np.cos(angi[jj % 8])
            tab[b + jj, 3, :] = np.sin(angi[jj % 8])
    return tab


def rot_mats():
    r = np.zeros((128, 2, 128), np.float32)
    for m in range(16):
        r[m + 16, 0, m] = -1.0
        r[m, 0, m + 16] = 1.0
    for b in (0, 64):
        for j in range(8):
            r[b + j + 8, 1, b + j] = -1.0
            r[b + j, 1, b + j + 8] = 1.0
    return r


def run_l2a(x_fm, inp):
    nc = get_nc("l2a", build_l2a)
    common = {"gains": gains_layout(inp["norm_gains"][2]), "rot": rot_mats(), "w_in": inp["attn_w_in"][0]}
    maps = []
    for core in range(NCORE):
        m = dict(common)
        m.update({"xin": seg_fm(x_fm, core), "rope": np.stack([rope_tables(s) for s in seg_of(core)])})
        maps.append(m)
    outs = run(nc, maps)
    full = {}
    for name in ("q", "k", "v", "qi"):
        full[name] = gather_fm(outs, name)
    for name in ("ki", "wi"):
        a0 = outs[0][name]
        f = np.empty((a0.shape[1], L), a0.dtype)
        for core in range(NCORE):
            for i, s in enumerate(seg_of(core)):
                f[:, s * SEG:(s + 1) * SEG] = outs[core][name][i]
        full[name] = f
    return full


NQB = 8
BIS_ITERS = 14
TOPK = 256


def build_l2b(nqb=NQB, stop_after=None):
    P = Prog()
    P.bank_list = [4, 5, 6, 7]
    nc, S = P.nc, P.S
    q_d = P.dram_in("q", [NQB, 128, 16, 128], BF16)
    qi_d = P.dram_in("qi", [NQB, 128, 8, 128], BF16)
    wi_d = P.dram_in("wi", [NQB, 128, 16])
    ki_d = P.dram_in("ki", [128, L], BF16)
    k_d = P.dram_in("k", [4, 128, L], BF16)
    v_d = P.dram_in("v", [4, 128, 64, 128], BF16)
    cm_d = P.dram_in("cmask", [128, 1024])
    id_d = P.dram_in("ident", [128, 128])
    o_d = P.dram_out("o", [NQB, 4, 128, 512], BF16)

    Ibuf = P.sb("I", [128, L], F32)
    Rb = P.sb("R", [128, 4, 512], BF16)
    dg = P.sb("dg", [128, 1, 16, 128], BF16)
    Mb = P.sb("Mb", [128, L], BF16)
    MbT = P.sb("MbT", [128, 64, 128], BF16)
    ki = P.sb("ki_sb", [128, L], BF16)
    kTb = P.sb("kTb", [128, 2, L], BF16)
    Vb = P.sb("Vb", [128, 2, 64, 128], BF16)
    qsb = P.sb("q_sb", [128, 1, 16, 128], BF16)
    qisb = P.sb("qi_sb", [128, 2, 8, 128], BF16)
    PT = P.sb("PT", [128, 2, 512], BF16)
    rden = P.sb("rden", [128, 512], F32)
    ob = P.sb("ob", [128, 2, 512], BF16)
    cmask = P.sb("cmask_sb", [128, 1024], F32)
    identf = P.sb("identf", [128, 128], F32)
    identb = P.sb("identb", [128, 128], BF16)
    wis = P.sb("wis", [128, 2, 16], F32)
    wabs = P.sb("wabs", [128, 2, 16], F32)
    wsgn = P.sb("wsgn", [128, 2, 16], F32)
    sm = P.sb("sm", [128, 16], F32)

    P.load(cmask[:], cm_d[:, :], "cmask")
    P.load(identf[:], id_d[:, :], "identf")
    S.op("dve", lambda: nc.vector.tensor_copy(out=identb[:], in_=identf[:]), reads=["identf"], writes=["identb"])
    for g in range(4):
        P.load(ki[:, g * 2048:(g + 1) * 2048], ki_d[:, g * 2048:(g + 1) * 2048], ("ki", g), lane="ki")
    BD = [0, 1]
    BO = [2, 3]
    IACC = [0, 1, 2, 3]
    acc_i = 0
    for i in range(nqb):
        nk = 1024 * (i + 1)
        ng = nk // 512
        nkb = nk // 128
        qs = i % 2
        P.load(qsb[:, 0], q_d[i], ("q", 0), lane=f"qin{i}")
        P.load(qisb[:, qs], qi_d[i], ("qi", qs), lane=f"qin{i}")
        P.load(wis[:, qs, :], wi_d[i], ("wi", qs), lane=f"qin{i}")
        S.op("dve", (lambda qs=qs: nc.vector.tensor_scalar(out=wabs[:, qs, :], in0=wis[:, qs, :], scalar1=-1.0, scalar2=None,
                                                           op0=ALU.mult)),
             reads=[("wi", qs)], writes=[("wabs", qs)])
        S.op("dve", (lambda qs=qs: nc.vector.tensor_tensor(out=wabs[:, qs, :], in0=wabs[:, qs, :], in1=wis[:, qs, :],
                                                           op=ALU.max)),
             reads=[("wi", qs), ("wabs", qs)], writes=[("wabs", qs)])
        S.op("dve", (lambda qs=qs: nc.vector.tensor_scalar(out=wsgn[:, qs, :], in0=wis[:, qs, :], scalar1=0.0, scalar2=2.0,
                                                           op0=ALU.is_ge, op1=ALU.mult)),
             reads=[("wi", qs)], writes=[("wsgn", qs)])
        S.op("dve", (lambda qs=qs: nc.vector.tensor_scalar(out=wsgn[:, qs, :], in0=wsgn[:, qs, :], scalar1=-1.0,
                                                           scalar2=None, op0=ALU.add)),
             reads=[("wsgn", qs)], writes=[("wsgn", qs)])
        for h in range(16):
            S.op("dve", (lambda h=h, qs=qs: nc.vector.tensor_scalar(
                out=dg[:, 0, h, :], in0=identf[:], scalar1=wsgn[:, qs, h:h + 1], scalar2=None, op0=ALU.mult)),
                reads=["identf", ("wsgn", qs)], writes=[("dg", 0)])
        for G in range(ng):
            ab = IACC[G % 4]
            pend = None
            for h in range(16):
                b = P.bank()
                p0 = 64 * (h % 2)
                ch = h // 2
                S.op("pe", (lambda b=b, p0=p0, ch=ch, G=G, qs=qs: nc.tensor.matmul(
                    P.banks[b][:, :512], lhsT=qisb[p0:p0 + 64, qs, ch, :], rhs=ki[p0:p0 + 64, G * 512:(G + 1) * 512],
                    start=True, stop=True)),
                    reads=[("qi", qs), ("ki", G // 4)], writes=[("ps", b)])
                r = h % 4
                S.op("act", (lambda b=b, r=r, h=h, qs=qs: nc.scalar.activation(
                    out=Rb[:, r, :], in_=P.banks[b][:, :512], func=AF.Relu, scale=wabs[:, qs, h:h + 1])),
                    reads=[("ps", b), ("wabs", qs)], writes=[("R", r)])
                if pend is not None:
                    pend()

                def accmm(ab=ab, r=r, h=h, qs=qs):
                    S.op("pe", (lambda: nc.tensor.matmul(P.banks[ab][:, :512], lhsT=dg[:, 0, h, :], rhs=Rb[:, r, :],
                                                         start=(h == 0), stop=(h == 15))),
                         reads=[("R", r), ("dg", 0)], writes=[("ps", ab)])
                pend = accmm
            pend()
            S.op("dve", (lambda ab=ab, G=G: nc.vector.tensor_copy(out=Ibuf[:, G * 512:(G + 1) * 512],
                                                                   in_=P.banks[ab][:, :512])),
                 reads=[("ps", ab)], writes=[("I", G)])
        Ikeys = [("I", G) for G in range(ng)]
        if stop_after == 'A':
            continue
        S.op("dve", (lambda nk=nk: nc.vector.tensor_reduce(out=sm[:, 0:1], in_=Ibuf[:, :nk], axis=AX.X, op=ALU.min)),
             reads=Ikeys, writes=[("sm", 0)])
        for G in (ng - 2, ng - 1):
            o = (G - (ng - 2)) * 512
            S.op("dve", (lambda G=G, o=o: nc.vector.tensor_tensor(
                out=Ibuf[:, G * 512:(G + 1) * 512], in0=Ibuf[:, G * 512:(G + 1) * 512], in1=cmask[:, o:o + 512], op=ALU.add)),
                reads=[("I", G), "cmask", ("sm", 0)], writes=[("I", G)])
        S.op("dve", (lambda nk=nk: nc.vector.tensor_reduce(out=sm[:, 1:2], in_=Ibuf[:, :nk], axis=AX.X, op=ALU.max)),
             reads=Ikeys, writes=[("sm", 1)])
        S.op("dve", lambda: nc.vector.tensor_tensor(out=sm[:, 1:2], in0=sm[:, 1:2], in1=sm[:, 0:1], op=ALU.subtract),
             reads=[("sm", 0), ("sm", 1)], writes=[("sm", 1)])
        for it in range(BIS_ITERS):
            S.op("dve", lambda: nc.vector.tensor_scalar(out=sm[:, 1:2], in0=sm[:, 1:2], scalar1=0.5, scalar2=None,
                                                        op0=ALU.mult),
                 reads=[("sm", 1)], writes=[("sm", 1)])
            S.op("dve", lambda: nc.vector.tensor_tensor(out=sm[:, 2:3], in0=sm[:, 0:1], in1=sm[:, 1:2], op=ALU.add),
                 reads=[("sm", 0), ("sm", 1)], writes=[("sm", 2)])
            S.op("dve", (lambda nk=nk: nc.vector.tensor_scalar(
                out=Mb[:, :nk], in0=Ibuf[:, :nk], scalar1=sm[:, 2:3], scalar2=0.0, op0=ALU.is_ge, op1=ALU.add,
                accum_out=sm[:, 3:4])),
                reads=Ikeys + [("sm", 2)], writes=[("sm", 3), "Mb"])
            S.op("dve", lambda: nc.vector.tensor_scalar(out=sm[:, 4:5], in0=sm[:, 3:4], scalar1=float(TOPK) - 0.5,
                                                        scalar2=None, op0=ALU.is_ge),
                 reads=[("sm", 3)], writes=[("sm", 4)])
            S.op("dve", lambda: nc.vector.tensor_tensor(out=sm[:, 4:5], in0=sm[:, 4:5], in1=sm[:, 1:2], op=ALU.mult),
                 reads=[("sm", 4), ("sm", 1)], writes=[("sm", 4)])
            S.op("dve", lambda: nc.vector.tensor_tensor(out=sm[:, 0:1], in0=sm[:, 0:1], in1=sm[:, 4:5], op=ALU.add),
                 reads=[("sm", 0), ("sm", 4)], writes=[("sm", 0)])
        if stop_after == 'B':
            continue
        S.op("dve", (lambda nk=nk: nc.vector.tensor_scalar(
            out=Mb[:, :nk], in0=Ibuf[:, :nk], scalar1=sm[:, 0:1], scalar2=-1.0e5, op0=ALU.is_lt, op1=ALU.mult)),
            reads=Ikeys + [("sm", 0)], writes=["Mb"])
        for g4 in range(nkb // 4):
            b = P.bank()
            for j in range(4):
                kb = g4 * 4 + j
                S.op("pe", (lambda b=b, j=j, kb=kb: nc.tensor.matmul(
                    P.banks[b][:, j * 128:(j + 1) * 128], lhsT=Mb[:, kb * 128:(kb + 1) * 128], rhs=identb[:],
                    start=True, stop=True)),
                    reads=["Mb", "identb"], writes=[("ps", b)])
            S.op("act", (lambda b=b, g4=g4: nc.scalar.copy(
                out=MbT[:, g4 * 4:(g4 + 1) * 4, :], in_=P.banks[b][:, :512].rearrange("p (j q) -> p j q", q=128))),
                reads=[("ps", b)], writes=[("MbT", g4)])
        if stop_after == 'C':
            continue
        for kv in range(4):
            a = acc_i % 2
            acc_i += 1
            P.load(kTb[:, a, :nk], k_d[kv, :, :nk], ("kT", a))
            P.load(Vb[:, a, :nkb, :], v_d[kv, :, :nkb, :], ("V", a))
            bd, bo = BD[a], BO[a]
            for kb in range(nkb):
                b = P.bank()
                pt = kb % 2
                S.op("pe", (lambda b=b, kb=kb, a=a, kv=kv, qs=qs: nc.tensor.matmul(
                    P.banks[b][:, :512], lhsT=kTb[:, a, kb * 128:(kb + 1) * 128], rhs=qsb[:, 0, 4 * kv:4 * kv + 4, :],
                    start=True, stop=False)),
                    reads=[("kT", a), ("q", 0)], writes=[("ps", b)])
                for hh in range(4):
                    S.op("pe", (lambda b=b, kb=kb, hh=hh: nc.tensor.matmul(
                        P.banks[b][:, hh * 128:(hh + 1) * 128], lhsT=identb[:], rhs=MbT[:, kb, :],
                        start=False, stop=(hh == 3))),
                        reads=[("MbT", kb // 4), "identb"], writes=[("ps", b)])
                if stop_after == 'D1':
                    continue
                S.op("act", (lambda b=b, pt=pt: nc.scalar.activation(out=PT[:, pt, :], in_=P.banks[b][:, :512], func=AF.Exp,
                                                                     scale=128.0 ** -0.5)),
                     reads=[("ps", b)], writes=[("PT", pt)])
                if stop_after == 'D2':
                    continue
                S.op("pe", (lambda pt=pt, bd=bd, kb=kb, nkb=nkb: nc.tensor.matmul(
                    P.banks[bd][:, :512], lhsT=P.ones[:], rhs=PT[:, pt, :], start=(kb == 0), stop=(kb == nkb - 1))),
                    reads=[("PT", pt), "ones"], writes=[("ps", bd)])
                S.op("pe", (lambda pt=pt, bo=bo, kb=kb, nkb=nkb, a=a: nc.tensor.matmul(
                    P.banks[bo][:, :512], lhsT=Vb[:, a, kb, :], rhs=PT[:, pt, :], start=(kb == 0), stop=(kb == nkb - 1))),
                    reads=[("PT", pt), ("V", a)], writes=[("ps", bo)])
            if stop_after in ('D1', 'D2', 'D3'):
                continue
            S.op("act", (lambda bd=bd: nc.scalar.copy(out=rden[:], in_=P.banks[bd][:, :512])),
                 reads=[("ps", bd)], writes=["rden"])
            S.op("dve", lambda: nc.vector.reciprocal(out=rden[:], in_=rden[:]), reads=["rden"], writes=["rden"])
            S.op("dve", (lambda bo=bo, a=a: nc.vector.tensor_tensor(out=ob[:, a, :], in0=P.banks[bo][:, :512], in1=rden[:],
                                                                    op=ALU.mult)),
                 reads=[("ps", bo), "rden"], writes=[("ob", a)])
            P.store(o_d[i, kv], ob[:, a, :], [("ob", a)], lane=f"o{a}", batch=False, eng="pool")
    return P.finish()


def run_l2b(pr):
    nc = get_nc("l2b", build_l2b)
    q, k, v, qi, ki, wi = pr["q"], pr["k"], pr["v"], pr["qi"], pr["ki"], pr["wi"]
    kfull = np.ascontiguousarray(k.reshape(4, 128, L))
    vfull = np.ascontiguousarray(v.reshape(4, 128, 64, 128).transpose(0, 3, 2, 1))
    ki2 = np.ascontiguousarray(np.concatenate([ki, ki], axis=0))
    common = {"ki": ki2, "k": kfull, "v": vfull, "ident": np.eye(128, dtype=np.float32)}
    maps = []
    for core in range(NCORE):
        qb = [8 * i + core for i in range(NQB)]
        qc = np.stack([q[:, :, b * 128:(b + 1) * 128].transpose(1, 0, 2) for b in qb])
        qic = np.stack([qi[:, :, b * 128:(b + 1) * 128].transpose(1, 0, 2) for b in qb])
        wic = np.stack([wi[:, b * 128:(b + 1) * 128].T for b in qb]).astype(np.float32)
        cm = np.zeros((128, 1024), np.float32)
        kpos = np.arange(1024)[None, :]
        qpos = (128 * core + np.arange(128))[:, None]
        cm[kpos > qpos] = -1.0e30
        m = dict(common)
        m.update({"q": np.ascontiguousarray(qc), "qi": np.ascontiguousarray(qic), "wi": np.ascontiguousarray(wic),
                  "cmask": cm})
        maps.append(m)
    outs = run(nc, maps)
    o_fm = np.empty((16, 128, L), outs[0]["o"].dtype)
    for core in range(NCORE):
        o = outs[core]["o"].reshape(NQB, 4, 128, 4, 128)
        for i in range(NQB):
            b = 8 * i + core
            for kv in range(4):
                for h in range(4):
                    o_fm[4 * kv + h, :, b * 128:(b + 1) * 128] = o[i, kv, :, h, :]
    return o_fm


def build_tail(glu, emit_u=False):
    P = Prog()
    nc, S = P.nc, P.S
    N = SEG
    xin_d = P.dram_in("xin", [2, DC, 128, N])
    a_d = P.dram_in("a", [2, DC, 128, N], BF16)
    gains_d = P.dram_in("gains", [128, 5 * DC])
    if emit_u:
        u_d = P.dram_out("u", [2, DC, 128, N], BF16)
    M = 2 * D if glu else D
    w_mix = P.dram_in("w_mix", [M // 512, 128, 8192])
    w_up = P.dram_in("w_up", [16, 128, 8192])
    w_down = P.dram_in("w_down", [16, 128, 8192])
    out_d = P.dram_out("out", [2, DC, 128, N])

    P.consts()
    xin = P.sb("xin", [128, DC, N], F32)
    xn = P.sb("xn", [128, DC, N], BF16)
    tmpA = P.sb("tmpA", [128, DC, N], F32)
    hbuf = P.sb("hbuf", [128, 64, N], BF16)
    rstd = P.sb("rstd", [128, N], F32)
    tmp2 = P.sb("tmp2", [128, 4, N], F32)
    gains = P.sb("gains_sb", [128, 5 * DC], F32)
    P.load(gains[:], gains_d[:, :], "gains")
    for t in range(2):
        for c in range(DC):
            P.load(xin[:, c, :], xin_d[t, c], ("xin", c), lane=f"xin{t}")
            P.load(xn[:, c, :], a_d[t, c], ("xn", c), lane=f"a{t}")
        if glu:
            groups = [[(256 * g, 256), (2048 + 256 * g, 256)] for g in range(8)]

            def evac(gi, bks):
                for j in range(2):
                    c = gi * 2 + j
                    ba, bg = bks[j], bks[2 + j]
                    S.op("act", (lambda bg=bg, j=j: nc.scalar.activation(out=tmp2[:, j, :], in_=P.banks[bg][:, :N],
                                                                         func=AF.Sigmoid)),
                         reads=[("ps", bg)], writes=[("tmp2", j)])
                    S.op("dve", (lambda ba=ba, c=c, j=j: nc.vector.tensor_tensor(
                        out=tmpA[:, c, :], in0=P.banks[ba][:, :N], in1=tmp2[:, j, :], op=ALU.mult)),
                        reads=[("ps", ba), ("tmp2", j)], writes=[("tmpA", c)])
            P.linear(w_mix, D, M, lambda kk: xn[:, kk, :], lambda kk: [("xn", kk)], N, evac, col_groups=groups, pre=True)
        else:
            def evac(gi, bks):
                for mb in range(4):
                    c = gi * 4 + mb
                    S.op("act", (lambda b=bks[mb], c=c: nc.scalar.copy(out=tmpA[:, c, :], in_=P.banks[b][:, :N])),
                         reads=[("ps", bks[mb])], writes=[("tmpA", c)])
            P.linear(w_mix, D, M, lambda kk: xn[:, kk, :], lambda kk: [("xn", kk)], N, evac, pre=True)
        P.rms_rstd(tmpA, "tmpA", 0, N, hbuf, "h", rstd, "rstd")
        P.norm_add(tmpA, "tmpA", N, gains, DC, rstd, "rstd", xin, "xin", 0, tmp2, "tmp2")
        P.mlp(xin, "xin", 0, N, gains, 2 * DC, 3 * DC, w_up, w_down, xn, "xn", hbuf, tmpA, rstd, tmp2)
        for c in range(DC):
            P.store(out_d[t, c], xin[:, c, :], [("xin", c)], lane=f"out{t}")
        if emit_u:
            P.rms_rstd(xin, "xin", 0, N, hbuf, "h", rstd, "rstd")
            P.norm_apply(xin, "xin", 0, N, gains, 4 * DC, rstd, "rstd", xn, "xn", 0)
            for c in range(DC):
                P.store(u_d[t, c], xn[:, c, :], [("xn", c)], lane=f"u{t}")
    return P.finish()


def run_tail(glu, a_fm, x_fm, inp, layer, w_mix, gnext=None):
    emit_u = gnext is not None
    nc = get_nc(("tail", glu, emit_u), lambda: build_tail(glu, emit_u))
    g5 = np.zeros((128, 5 * DC), np.float32)
    g5[:, :4 * DC] = gains_layout(inp["norm_gains"][layer])
    if emit_u:
        g5[:, 4 * DC:] = col_layout(gnext)
    common = {"gains": g5, "w_mix": slotify(np.asarray(w_mix, dtype=np.float32), GLU_GROUPS if glu else None),
              "w_up": slot_cached(inp, "mlp_w_up", layer), "w_down": slot_cached(inp, "mlp_w_down", layer)}
    maps = []
    for core in range(NCORE):
        m = dict(common)
        m.update({"xin": seg_fm(x_fm, core), "a": seg_fm(a_fm, core)})
        maps.append(m)
    outs = run(nc, maps)
    if emit_u:
        return gather_fm(outs), gather_fm(outs, "u")
    return gather_fm(outs)


TWO_PI = 6.283185307179586
NTT = L // 128


def build_l3b(ntiles=NTT, stage=9):
    P = Prog()
    P.bank_list = [4, 5, 6, 7]
    nc, S = P.nc, P.S
    u_d = P.dram_in("u", [2, 128, L], BF16)
    lam_d = P.dram_in("lam", [128, 3, 8])
    bblk_d = P.dram_in("bblk", [128, 2, 2, 512])
    cpad_d = P.dram_in("cpad", [128, 8, 2, 128])
    dsk_d = P.dram_in("dskip", [128, 2])
    tri_d = P.dram_in("tri", [128, 128])
    id_d = P.dram_in("ident", [128, 128])
    y_d = P.dram_out("y", [2, 128, L], BF16)

    lam = P.sb("lam", [128, 3, 8], F32)
    sc = P.sb("sc", [128, 40, 8], F32)
    ki32 = P.sb("ki32", [128, 8], mybir.dt.int32)
    bblkf = P.sb("bblkf", [128, 2, 2, 512], F32)
    bblk = P.sb("bblk_sb", [128, 2, 2, 512], BF16)
    cpadf = P.sb("cpadf", [128, 8, 2, 128], F32)
    cpad = P.sb("cpad_sb", [128, 8, 2, 128], BF16)
    dsk = P.sb("dsk", [128, 2], F32)
    trif = P.sb("trif", [128, 128], F32)
    tri = P.sb("tri_sb", [128, 128], BF16)
    ident = P.sb("ident_sb", [128, 128], F32)
    T2 = P.sb("T2", [128, 2, 8, 128], F32)
    T1s = P.sb("T1s", [128, 2, 8, 128], F32)
    T1 = P.sb("T1", [128, 2, 8, 128], F32)
    tmpp = P.sb("tmpp", [128, 2, 128], F32)
    usb = P.sb("usb", [128, 2, 2, 1024], BF16)
    ysb = P.sb("ysb", [128, 2, 2, 1024], BF16)
    Vb = P.sb("Vb", [128, 2, 2, 1024], BF16)
    Wp = P.sb("Wp", [128, 2, 8, 128], F32)
    hb = P.sb("hb", [128, 2, 8, 128], BF16)
    mm = P.sb("mm", [128, 4, 512], F32)
    gt = P.sb("gt", [128, 4, 128], F32)
    ahp = P.sb("ahp", [128, 2, 8], F32)
    t6 = P.sb("t6", [128, 6, 8], F32)

    P.load(lam[:], lam_d[:, :, :], "lam")
    P.load(bblkf[:], bblk_d[:, :, :, :], "bblkf")
    P.load(cpadf[:], cpad_d[:, :, :, :], "cpadf")
    P.load(dsk[:], dsk_d[:, :], "dsk")
    P.load(trif[:], tri_d[:, :], "trif")
    P.load(ident[:], id_d[:, :], "ident")
    S.op("dve", lambda: nc.vector.tensor_copy(out=bblk[:], in_=bblkf[:]), reads=["bblkf"], writes=["bblk"])
    S.op("dve", lambda: nc.vector.tensor_copy(out=tri[:], in_=trif[:]), reads=["trif"], writes=["tri"])
    S.op("dve", lambda: nc.vector.tensor_copy(out=cpad[:, :, 0, :], in_=cpadf[:, :, 0, :]), reads=["cpadf"], writes=["cpad0"])
    S.op("dve", lambda: nc.vector.tensor_scalar(out=cpad[:, :, 1, :], in0=cpadf[:, :, 1, :], scalar1=-1.0, scalar2=None,
                                                op0=ALU.mult), reads=["cpadf"], writes=["cpad1"])

    def R(i):
        return sc[:, i, :]

    def tt(o, a, b, op):
        S.op("dve", lambda: nc.vector.tensor_tensor(out=R(o), in0=R(a), in1=R(b), op=op),
             reads=[("sc", a), ("sc", b)], writes=[("sc", o)])

    def ts(o, a, s1, op0, s2=None, op1=None):
        if op1 is None:
            S.op("dve", lambda: nc.vector.tensor_scalar(out=R(o), in0=R(a), scalar1=s1, scalar2=None, op0=op0),
                 reads=[("sc", a)], writes=[("sc", o)])
        else:
            S.op("dve", lambda: nc.vector.tensor_scalar(out=R(o), in0=R(a), scalar1=s1, scalar2=s2, op0=op0, op1=op1),
                 reads=[("sc", a)], writes=[("sc", o)])

    def act(o, a, func, scale=1.0):
        S.op("act", lambda: nc.scalar.activation(out=R(o), in_=R(a), func=func, scale=scale),
             reads=[("sc", a)], writes=[("sc", o)])

    def wrap_pi(o, a, t1, t2):
        ts(t1, a, 1.0 / TWO_PI, ALU.mult)
        S.op("dve", lambda: nc.vector.tensor_copy(out=ki32[:], in_=R(t1)), reads=[("sc", t1)], writes=["ki32"])
        S.op("dve", lambda: nc.vector.tensor_copy(out=R(t1), in_=ki32[:]), reads=["ki32"], writes=[("sc", t1)])
        S.op("dve", lambda: nc.vector.scalar_tensor_tensor(out=R(o), in0=R(t1), scalar=-TWO_PI, in1=R(a),
                                                           op0=ALU.mult, op1=ALU.add),
             reads=[("sc", t1), ("sc", a)], writes=[("sc", o)])
        ts(t2, o, 3.141592653589793, ALU.is_gt, -TWO_PI, ALU.mult)
        tt(o, o, t2, ALU.add)
        ts(t2, o, -3.141592653589793, ALU.is_lt, TWO_PI, ALU.mult)
        tt(o, o, t2, ALU.add)

    S.op("dve", lambda: nc.vector.tensor_copy(out=sc[:, 0:3, :], in_=lam[:]), reads=["lam"],
         writes=[("sc", 0), ("sc", 1), ("sc", 2)])
    act(2, 2, AF.Exp)
    tt(3, 0, 2, ALU.mult)
    tt(4, 1, 2, ALU.mult)
    act(5, 3, AF.Exp)
    wrap_pi(12, 4, 13, 14)
    act(15, 12, AF.Sin)
    ts(16, 4, 1.5707963267948966, ALU.add)
    wrap_pi(17, 16, 13, 14)
    act(18, 17, AF.Sin)
    tt(6, 5, 18, ALU.mult)
    tt(7, 5, 15, ALU.mult)
    act(19, 3, AF.Exp, scale=-1.0)
    tt(8, 19, 18, ALU.mult)
    tt(9, 19, 15, ALU.mult)
    ts(9, 9, -1.0, ALU.mult)
    ts(20, 6, -1.0, ALU.add)
    tt(21, 0, 0, ALU.mult)
    tt(22, 1, 1, ALU.mult)
    tt(21, 21, 22, ALU.add)
    S.op("dve", lambda: nc.vector.reciprocal(out=R(21), in_=R(21)), reads=[("sc", 21)], writes=[("sc", 21)])
    tt(22, 20, 0, ALU.mult)
    tt(23, 7, 1, ALU.mult)
    tt(22, 22, 23, ALU.add)
    tt(10, 22, 21, ALU.mult)
    tt(22, 7, 0, ALU.mult)
    tt(23, 20, 1, ALU.mult)
    tt(22, 22, 23, ALU.subtract)
    tt(11, 22, 21, ALU.mult)

    def power_table(T, base_r, base_i, init_r, init_i, pr, pi, q1, q2):
        tt(pr, base_r, base_r, ALU.max)
        tt(pi, base_i, base_i, ALU.max)
        for j in range(8):
            if init_r is None:
                S.op("dve", (lambda j=j: nc.vector.memset(T[:, 0, j, 0:1], 1.0)), writes=[("T", id(T), j)])
                S.op("dve", (lambda j=j: nc.vector.memset(T[:, 1, j, 0:1], 0.0)), writes=[("T", id(T), j)])
            else:
                S.op("dve", (lambda j=j: nc.vector.tensor_copy(out=T[:, 0, j, 0:1], in_=sc[:, init_r, j:j + 1])),
                     reads=[("sc", init_r)], writes=[("T", id(T), j)])
                S.op("dve", (lambda j=j: nc.vector.tensor_copy(out=T[:, 1, j, 0:1], in_=sc[:, init_i, j:j + 1])),
                     reads=[("sc", init_i)], writes=[("T", id(T), j)])
        for k in range(7):
            w = 1 << k
            for j in range(8):
                key = ("T", id(T), j)
                S.op("dve", (lambda j=j, w=w: nc.vector.tensor_scalar(
                    out=tmpp[:, 0, :w], in0=T[:, 1, j, 0:w], scalar1=sc[:, pi, j:j + 1], scalar2=None, op0=ALU.mult)),
                    reads=[key, ("sc", pi)], writes=[("tmpp", 0)])
                S.op("dve", (lambda j=j, w=w: nc.vector.scalar_tensor_tensor(
                    out=T[:, 0, j, w:2 * w], in0=T[:, 0, j, 0:w], scalar=sc[:, pr, j:j + 1], in1=tmpp[:, 0, :w],
                    op0=ALU.mult, op1=ALU.subtract)),
                    reads=[key, ("sc", pr), ("tmpp", 0)], writes=[key])
                S.op("dve", (lambda j=j, w=w: nc.vector.tensor_scalar(
                    out=tmpp[:, 1, :w], in0=T[:, 1, j, 0:w], scalar1=sc[:, pr, j:j + 1], scalar2=None, op0=ALU.mult)),
                    reads=[key, ("sc", pr)], writes=[("tmpp", 1)])
                S.op("dve", (lambda j=j, w=w: nc.vector.scalar_tensor_tensor(
                    out=T[:, 1, j, w:2 * w], in0=T[:, 0, j, 0:w], scalar=sc[:, pi, j:j + 1], in1=tmpp[:, 1, :w],
                    op0=ALU.mult, op1=ALU.add)),
                    reads=[key, ("sc", pi), ("tmpp", 1)], writes=[key])
            tt(q1, pr, pr, ALU.mult)
            tt(q2, pi, pi, ALU.mult)
            tt(q2, q1, q2, ALU.subtract)
            tt(q1, pr, pi, ALU.mult)
            ts(pi, q1, 2.0, ALU.mult)
            tt(pr, q2, q2, ALU.max)

    power_table(T2, 6, 7, None, None, 24, 25, 26, 27)
    power_table(T1s, 8, 9, 10, 11, 28, 29, 26, 27)
    for ri in range(2):
        for j4 in range(2):
            b = P.bank()
            for jj in range(4):
                j = j4 * 4 + jj
                S.op("pe", (lambda b=b, jj=jj, j=j, ri=ri: nc.tensor.matmul(
                    P.banks[b][:, jj * 128:(jj + 1) * 128], lhsT=T1s[:, ri, j, :], rhs=ident[:], start=True, stop=True)),
                    reads=[("T", id(T1s), j), "ident"], writes=[("ps", b)])
            S.op("act", (lambda b=b, ri=ri, j4=j4: nc.scalar.copy(
                out=T1[:, ri, j4 * 4:(j4 + 1) * 4, :], in_=P.banks[b][:, :512].rearrange("p (j n) -> p j n", n=128))),
                reads=[("ps", b)], writes=[("T1", ri, j4)])
    S.op("dve", lambda: nc.vector.memset(ahp[:], 0.0), writes=["ahp"])
    T2keys = [("T", id(T2), j) for j in range(8)]

    for it in range(ntiles):
        big = it // 8
        ub = big % 2
        if it % 8 == 0:
            for fc in range(2):
                P.load(usb[:, ub, fc, :], u_d[fc, :, big * 1024:(big + 1) * 1024], ("u", ub, fc), lane=f"u{big}")
        tcol = (it % 8) * 128
        vb = it % 2
        xb = {}
        for fc in range(2):
            for ri in range(2):
                b = P.bank()
                xb[(fc, ri)] = b
                S.op("pe", (lambda b=b, fc=fc, ri=ri, ub=ub, tcol=tcol: nc.tensor.matmul(
                    P.banks[b][:, :512], lhsT=usb[:, ub, fc, tcol:tcol + 128], rhs=bblk[:, fc, ri, :],
                    start=True, stop=True)),
                    reads=[("u", ub, fc), "bblk"], writes=[("ps", b)])
        for fc in range(2):
            br_, bi_ = xb[(fc, 0)], xb[(fc, 1)]
            t1r = T1[:, 0, fc * 4:(fc + 1) * 4, :]
            t1i = T1[:, 1, fc * 4:(fc + 1) * 4, :]
            rk = [("T1", 0, fc), ("T1", 1, fc)]

            def pmul(o, b, tab):
                S.op("dve", (lambda o=o, b=b, tab=tab: nc.vector.tensor_tensor(
                    out=mm[:, o, :].rearrange("p (j n) -> p j n", n=128),
                    in0=P.banks[b][:, :512].rearrange("p (j n) -> p j n", n=128), in1=tab, op=ALU.mult)),
                    reads=[("ps", b)] + rk, writes=[("mm", o)])
            pmul(0, br_, t1r)
            pmul(1, bi_, t1i)
            S.op("dve", (lambda fc=fc, vb=vb: nc.vector.tensor_tensor(
                out=Vb[:, vb, 0, fc * 512:(fc + 1) * 512], in0=mm[:, 0, :], in1=mm[:, 1, :], op=ALU.subtract)),
                reads=[("mm", 0), ("mm", 1)], writes=[("V", vb, 0, fc)])
            pmul(2, br_, t1i)
            pmul(3, bi_, t1r)
            S.op("dve", (lambda fc=fc, vb=vb: nc.vector.tensor_tensor(
                out=Vb[:, vb, 1, fc * 512:(fc + 1) * 512], in0=mm[:, 2, :], in1=mm[:, 3, :], op=ALU.add)),
                reads=[("mm", 2), ("mm", 3)], writes=[("V", vb, 1, fc)])
        if stage < 2:
            continue
        for ri in range(2):
            for j4 in range(2):
                b = P.bank()
                for jj in range(4):
                    j = j4 * 4 + jj
                    S.op("pe", (lambda b=b, jj=jj, j=j, ri=ri, vb=vb: nc.tensor.matmul(
                        P.banks[b][:, jj * 128:(jj + 1) * 128], lhsT=Vb[:, vb, ri, j * 128:(j + 1) * 128], rhs=tri[:],
                        start=True, stop=True)),
                        reads=[("V", vb, ri, j // 4), "tri"], writes=[("ps", b)])
                for jj in range(4):
                    j = j4 * 4 + jj
                    S.op("dve", (lambda b=b, jj=jj, j=j, ri=ri: nc.vector.tensor_scalar(
                        out=Wp[:, ri, j, :], in0=P.banks[b][:, jj * 128:(jj + 1) * 128], scalar1=ahp[:, ri, j:j + 1],
                        scalar2=None, op0=ALU.add)),
                        reads=[("ps", b), "ahp"], writes=[("Wp", ri, j)])
        Wkeys = [("Wp", ri, j) for ri in range(2) for j in range(8)]
        if stage < 3:
            continue
        wl_r = Wp[:, 0, :, 127]
        wl_i = Wp[:, 1, :, 127]
        S.op("dve", lambda: nc.vector.tensor_tensor(out=t6[:, 0, :], in0=wl_r, in1=sc[:, 24, :], op=ALU.mult),
             reads=Wkeys + [("sc", 24)], writes=[("t6", 0)])
        S.op("dve", lambda: nc.vector.tensor_tensor(out=t6[:, 1, :], in0=wl_i, in1=sc[:, 25, :], op=ALU.mult),
             reads=Wkeys + [("sc", 25)], writes=[("t6", 1)])
        S.op("dve", lambda: nc.vector.tensor_tensor(out=t6[:, 2, :], in0=wl_r, in1=sc[:, 25, :], op=ALU.mult),
             reads=Wkeys + [("sc", 25)], writes=[("t6", 2)])
        S.op("dve", lambda: nc.vector.tensor_tensor(out=t6[:, 3, :], in0=wl_i, in1=sc[:, 24, :], op=ALU.mult),
             reads=Wkeys + [("sc", 24)], writes=[("t6", 3)])
        S.op("dve", lambda: nc.vector.tensor_tensor(out=ahp[:, 0, :], in0=t6[:, 0, :], in1=t6[:, 1, :], op=ALU.subtract),
             reads=[("t6", 0), ("t6", 1)] + Wkeys, writes=["ahp"])
        S.op("dve", lambda: nc.vector.tensor_tensor(out=ahp[:, 1, :], in0=t6[:, 2, :], in1=t6[:, 3, :], op=ALU.add),
             reads=[("t6", 2), ("t6", 3)], writes=["ahp"])
        if stage < 4:
            continue
        for hf in range(2):
            js = slice(hf * 4, hf * 4 + 4)
            wk = [("Wp", ri, j) for ri in range(2) for j in range(hf * 4, hf * 4 + 4)]

            def hmul(o, ri, ti, hf=hf, js=js, wk=wk):
                S.op("dve", (lambda: nc.vector.tensor_tensor(
                    out=mm[:, o, :].rearrange("p (j n) -> p j n", n=128), in0=Wp[:, ri, js, :], in1=T2[:, ti, js, :],
                    op=ALU.mult)),
                    reads=wk + T2keys, writes=[("mm", o)])
            hmul(0, 0, 0)
            hmul(1, 1, 1)
            S.op("dve", (lambda js=js: nc.vector.tensor_tensor(
                out=hb[:, 0, js, :], in0=mm[:, 0, :].rearrange("p (j n) -> p j n", n=128),
                in1=mm[:, 1, :].rearrange("p (j n) -> p j n", n=128), op=ALU.subtract)),
                reads=[("mm", 0), ("mm", 1)], writes=[("hb", 0, hf)])
            hmul(2, 0, 1)
            hmul(3, 1, 0)
            S.op("dve", (lambda js=js: nc.vector.tensor_tensor(
                out=hb[:, 1, js, :], in0=mm[:, 2, :].rearrange("p (j n) -> p j n", n=128),
                in1=mm[:, 3, :].rearrange("p (j n) -> p j n", n=128), op=ALU.add)),
                reads=[("mm", 2), ("mm", 3)], writes=[("hb", 1, hf)])
        if stage < 5:
            continue
        for fc in range(2):
            b = P.bank()
            n = 0
            for jj in range(4):
                j = fc * 4 + jj
                for ri in range(2):
                    S.op("pe", (lambda b=b, j=j, ri=ri, n=n: nc.tensor.matmul(
                        P.banks[b][:, :128], lhsT=cpad[:, j, ri, :], rhs=hb[:, ri, j, :], start=(n == 0), stop=(n == 7))),
                        reads=[("hb", ri, fc), f"cpad{ri}"], writes=[("ps", b)])
                    n += 1
            S.op("dve", (lambda b=b, fc=fc, ub=ub, tcol=tcol: nc.vector.scalar_tensor_tensor(
                out=gt[:, 0, :], in0=usb[:, ub, fc, tcol:tcol + 128], scalar=dsk[:, fc:fc + 1], in1=P.banks[b][:, :128],
                op0=ALU.mult, op1=ALU.add)),
                reads=[("ps", b), ("u", ub, fc), "dsk"], writes=[("gt", 0)])
            S.op("act", lambda: nc.scalar.activation(out=gt[:, 1, :], in_=gt[:, 0, :], func=AF.Square),
                 reads=[("gt", 0)], writes=[("gt", 1)])
            S.op("dve", lambda: nc.vector.tensor_scalar(out=gt[:, 1, :], in0=gt[:, 1, :], scalar1=0.044715, scalar2=1.0,
                                                        op0=ALU.mult, op1=ALU.add),
                 reads=[("gt", 1)], writes=[("gt", 1)])
            S.op("dve", lambda: nc.vector.tensor_tensor(out=gt[:, 2, :], in0=gt[:, 1, :], in1=gt[:, 0, :], op=ALU.mult),
                 reads=[("gt", 1), ("gt", 0)], writes=[("gt", 2)])
            S.op("act", lambda: nc.scalar.activation(out=gt[:, 3, :], in_=gt[:, 2, :], func=AF.Sigmoid, scale=1.5957691216),
                 reads=[("gt", 2)], writes=[("gt", 3)])
            S.op("dve", (lambda fc=fc, ub=ub, tcol=tcol: nc.vector.tensor_tensor(
                out=ysb[:, ub, fc, tcol:tcol + 128], in0=gt[:, 0, :], in1=gt[:, 3, :], op=ALU.mult)),
                reads=[("gt", 0), ("gt", 3)], writes=[("y", ub)])
        if it % 8 == 7 or it == ntiles - 1:
            for fc in range(2):
                P.store(y_d[fc, :, big * 1024:(big + 1) * 1024], ysb[:, ub, fc, :], [("y", ub)], lane=f"y{ub}", batch=False, eng="pool")
    return P.finish()


def run_l3b(u_fm, inp, ntiles=NTT, stage=9):
    nc = get_nc(("l3b", ntiles, stage), lambda: build_l3b(ntiles, stage))
    lr, li, ldt = inp["ssm_lambda_re"][0], inp["ssm_lambda_im"][0], inp["ssm_log_dt"][0]
    bre, bim = inp["ssm_b_re"][0], inp["ssm_b_im"][0]
    cre, cim = inp["ssm_c_re"][0], inp["ssm_c_im"][0]
    tri = np.triu(np.ones((128, 128), np.float32))
    maps = []
    for core in range(NCORE):
        g0 = 16 * core
        lam = np.zeros((128, 3, 8), np.float32)
        bblk = np.zeros((128, 2, 2, 512), np.float32)
        cpad = np.zeros((128, 8, 2, 128), np.float32)
        for j in range(8):
            for gp in range(2):
                g = g0 + 2 * j + gp
                rows = slice(gp * 64, gp * 64 + 64)
                lam[rows, 0, j] = lr[g]
                lam[rows, 1, j] = li[g]
                lam[rows, 2, j] = ldt[g]
                gl = (2 * j + gp) % 8
                fc = j // 4
                cpad[rows, j, 0, gl * 16:gl * 16 + 16] = cre[g].T
                cpad[rows, j, 1, gl * 16:gl * 16 + 16] = cim[g].T
                bblk[gl * 16:gl * 16 + 16, fc, 0, gl * 64:gl * 64 + 64] = bre[g].T
                bblk[gl * 16:gl * 16 + 16, fc, 1, gl * 64:gl * 64 + 64] = bim[g].T
        dsk = np.ascontiguousarray(inp["ssm_d"][0][256 * core:256 * core + 256].reshape(2, 128).T)
        maps.append({"u": np.ascontiguousarray(u_fm[2 * core:2 * core + 2]), "lam": lam, "bblk": bblk, "cpad": cpad,
                     "dskip": dsk, "tri": tri, "ident": np.eye(128, dtype=np.float32)})
    outs = run(nc, maps)
    y = np.empty((16, 128, L), outs[0]["y"].dtype)
    for core in range(NCORE):
        y[2 * core:2 * core + 2] = outs[core]["y"]
    return y


def seg_fm(a_fm, core):
    return np.ascontiguousarray(np.stack([a_fm[:, :, s * SEG:(s + 1) * SEG] for s in seg_of(core)]))


def gather_fm(outs, name="out"):
    C = outs[0][name].shape[1]
    full = np.empty((C, 128, L), outs[0][name].dtype)
    for core in range(NCORE):
        for i, s in enumerate(seg_of(core)):
            full[:, :, s * SEG:(s + 1) * SEG] = outs[core][name][i]
    return full


GLU_GROUPS = [[(256 * g, 256), (2048 + 256 * g, 256)] for g in range(8)]


def slotify(W, groups=None):
    K, M = W.shape
    if groups is None:
        groups = [[(m0, 512)] for m0 in range(0, M, 512)]
    nk = K // 2048
    out = np.empty((len(groups) * nk, 128, 16, 512), np.float32)
    for gi, grp in enumerate(groups):
        cols = np.concatenate([np.arange(m0, m0 + n) for (m0, n) in grp])
        sub = W[:, cols] if len(grp) > 1 else W[:, grp[0][0]:grp[0][0] + 512]
        sub = sub.reshape(nk, 16, 128, 512).transpose(0, 2, 1, 3)
        out[gi * nk:(gi + 1) * nk] = sub
    return out.reshape(len(groups) * nk, 128, 8192)


_SLOT_CACHE = {}


def slot_cached(inp, name, idx, groups=None):
    key = (name, idx, groups is not None)
    if key not in _SLOT_CACHE:
        _SLOT_CACHE[key] = slotify(np.asarray(inp[name][idx], dtype=np.float32), groups)
    return _SLOT_CACHE[key]


def gains_layout(norm_gains_i):
    return np.ascontiguousarray(np.concatenate([col_layout(norm_gains_i[j]) for j in range(4)], axis=1))


_NC_CACHE = {}


def get_nc(name, builder):
    if name not in _NC_CACHE:
        _NC_CACHE[name] = builder()
    return _NC_CACHE[name]


def run_layer0(x_fm, inp):
    nc = get_nc("l0", build_layer0)
    vecs = np.zeros((128, 6 * DC), np.float32)
    vecs[:, 0:DC] = col_layout(inp["conv_b_in"][0][:D])
    vecs[:, DC:2 * DC] = col_layout(inp["conv_b_in"][0][D:])
    vecs[:, 2 * DC:3 * DC] = col_layout(inp["conv_b_dw"][0])
    vecs[:, 3 * DC:4 * DC] = col_layout(inp["conv_ln_g"][0])
    vecs[:, 4 * DC:5 * DC] = col_layout(inp["conv_ln_b"][0])
    wdw = np.ascontiguousarray(inp["conv_w_dw"][0].T.reshape(DC, 128, 31).transpose(1, 0, 2))
    common = {"gains": gains_layout(inp["norm_gains"][0]), "vecs": vecs, "wdw": wdw,
              "ident": np.eye(128, dtype=np.float32),
              "w_in": slot_cached(inp, "conv_w_in", 0, GLU_GROUPS), "w_out": slot_cached(inp, "conv_w_out", 0),
              "w_up": slot_cached(inp, "mlp_w_up", 0), "w_down": slot_cached(inp, "mlp_w_down", 0)}
    maps = []
    for core in range(NCORE):
        xh = np.zeros((DC, 128, 2 * HALO), np.float32)
        hm = np.zeros((128, 2 * HALO), np.float32)
        for i, s in enumerate(seg_of(core)):
            if s > 0:
                xh[:, :, i * HALO:(i + 1) * HALO] = x_fm[:, :, s * SEG - HALO:s * SEG]
                hm[:, i * HALO:(i + 1) * HALO] = 1.0
        m = dict(common)
        m.update({"xin": seg_fm(x_fm, core), "xh": xh, "hmask": hm})
        maps.append(m)
    return gather_fm(run(nc, maps))


def halo_fm(x_fm, core, halo=HALO):
    xh = np.zeros((x_fm.shape[0], 128, 2 * halo), np.float32)
    for i, s in enumerate(seg_of(core)):
        if s > 0:
            xh[:, :, i * halo:(i + 1) * halo] = x_fm[:, :, s * SEG - halo:s * SEG]
    return xh


def run_layer1(x_fm, inp):
    nc = get_nc("l1", build_layer1)
    common = {"gains": gains_layout(inp["norm_gains"][1]), "pscale": col_layout(inp["pool_scale"][0]),
              "w_pool": np.ascontiguousarray(inp["pool_w"][0].reshape(D, 512)),
              "w_up": slot_cached(inp, "mlp_w_up", 1), "w_down": slot_cached(inp, "mlp_w_down", 1)}
    maps = []
    for core in range(NCORE):
        rdiv = np.empty((2, 128, 4, SEG), np.float32)
        for i, s in enumerate(seg_of(core)):
            tpos = np.arange(s * SEG, (s + 1) * SEG) + 1
            for g, w in enumerate(POOL_W):
                rdiv[i, :, g, :] = (1.0 / np.minimum(tpos, w)).astype(np.float32)[None, :]
        m = dict(common)
        m.update({"xin": seg_fm(x_fm, core), "xh": halo_fm(x_fm, core, 16), "rdiv": rdiv})
        maps.append(m)
    return gather_fm(run(nc, maps))


def kernel(**inputs):
    _SLOT_CACHE.clear()
    inp = {k: np.asarray(v) for k, v in inputs.items()}
    x_fm = to_fm(np.ascontiguousarray(inp["x"][0], dtype=np.float32))
    r0 = run_layer0(x_fm, inp)
    r1 = run_layer1(r0, inp)
    pr = run_l2a(r1, inp)
    o_fm = run_l2b(pr)
    r2, u_fm = run_tail(False, o_fm, r1, inp, 2, inp["attn_w_out"][0], gnext=inp["norm_gains"][3][0])
    y_fm = run_l3b(u_fm, inp)
    r3 = run_tail(True, y_fm, r2, inp, 3, inp["ssm_w_glu"][0])
    return from_fm(r3)[None].astype(np.float32)
```

```python
import numpy as np
from contextlib import ExitStack
import concourse.bass as bass
import concourse.mybir as mybir
from concourse.bass_utils import run_bass_kernel_spmd

F32 = mybir.dt.float32
BF16 = mybir.dt.bfloat16
AF = mybir.ActivationFunctionType
ALU = mybir.AluOpType
AX = mybir.AxisListType

D = 2048
DC = 16
L = 8192
NCORE = 8
SEG = 512
NSEG = 16
DFF = 8192
EPS = 1e-6
LN_EPS = 1e-5
HALO = 32


class _Op:
    __slots__ = ("eng", "fn", "deps", "inc", "val", "lane")

    def __init__(self, eng, fn, lane=None):
        self.eng = eng
        self.fn = fn
        self.deps = []
        self.inc = False
        self.val = 0
        self.lane = lane


class Sched:
    ENGS = ("pe", "act", "dve", "pool", "sp")

    def __init__(self, nc):
        self.nc = nc
        self.ops = {e: [] for e in self.ENGS}
        self.res = {}
        self.lanes = {}
        self.batch_lanes = set()

    def _dep(self, op, reads, writes):
        deps = []
        for k in reads:
            ent = self.res.get(k)
            if ent is not None and ent[0] is not None:
                deps.append(ent[0])
        for k in writes:
            ent = self.res.get(k)
            if ent is not None:
                if ent[0] is not None:
                    deps.append(ent[0])
                deps.extend(ent[1])
        for k in reads:
            ent = self.res.setdefault(k, [None, []])
            ent[1].append(op)
        for k in writes:
            self.res[k] = [op, []]
        seen = set()
        for d in deps:
            if d is op or id(d) in seen:
                continue
            seen.add(id(d))
            if d.lane is None and d.eng == op.eng and op.eng == "pe":
                continue
            if d.lane is not None and d.lane == op.lane and d.lane in self.batch_lanes:
                continue
            op.deps.append(d)

    def op(self, eng, fn, reads=(), writes=()):
        o = _Op(eng, fn)
        self.ops[eng].append(o)
        self._dep(o, reads, writes)
        return o

    def dma(self, eng, lane, fn, reads=(), writes=()):
        o = _Op(eng, fn, lane=lane)
        self.ops[eng].append(o)
        self.lanes.setdefault(lane, []).append(o)
        o.val = 16 * len(self.lanes[lane])
        self._dep(o, reads, writes)
        return o

    def emit(self, final_waits=()):
        nc = self.nc
        for ln in self.batch_lanes:
            tot = 16 * len(self.lanes[ln])
            for o in self.lanes[ln]:
                o.val = tot
        for e in self.ENGS:
            for o in self.ops[e]:
                for d in o.deps:
                    d.inc = True
        for e in self.ENGS:
            c = 0
            for o in self.ops[e]:
                if o.lane is None and o.inc:
                    c += 1
                    o.val = c
        with ExitStack() as st:
            sems = {}
            for e in self.ENGS:
                sems[e] = st.enter_context(nc.semaphore("s_" + e))
            for ln in self.lanes:
                sems[("lane", ln)] = st.enter_context(nc.semaphore("l_" + str(ln)))
            block = st.enter_context(nc.Block())
            hmap = {"pe": "tensor", "act": "scalar", "dve": "vector", "pool": "gpsimd", "sp": "sync"}

            def semof(o):
                return sems[("lane", o.lane)] if o.lane is not None else sems[o.eng]

            def body(e):
                def run(engh):
                    waited = {}
                    for o in self.ops[e]:
                        for d in o.deps:
                            s = semof(d)
                            key = id(s)
                            if waited.get(key, 0) >= d.val:
                                continue
                            waited[key] = d.val
                            engh.wait_ge(s, d.val)
                        ins = o.fn()
                        if o.lane is not None:
                            ins.then_inc(sems[("lane", o.lane)], 16)
                        elif o.inc:
                            ins.then_inc(sems[e], 1)
                    if e == "sp":
                        for o in final_waits:
                            engh.wait_ge(semof(o), o.val)
                return run

            for e in self.ENGS:
                getattr(block, hmap[e])(body(e))


class Prog:
    NSLOT = 2

    def __init__(self):
        self.nc = bass.Bass("TRN2", target_bir_lowering=False)
        self.st = ExitStack()
        self.S = Sched(self.nc)
        self.nbank = 0
        self.nslot = 0
        self.uid = 0
        nc = self.nc
        self.banks = [self.st.enter_context(nc.psum_tensor(f"ps{i}", [128, 512], F32)) for i in range(8)]
        self.slots = [self.st.enter_context(nc.sbuf_tensor(f"wslot{i}", [128, 16, 512], BF16))
                      for i in range(self.NSLOT)]
        self.ones = self.sb("ones", [128, 128], BF16)
        self.S.op("dve", lambda: nc.vector.memset(self.ones[:], 1.0), writes=["ones"])
        self.outs = []

    def sb(self, name, shape, dt):
        return self.st.enter_context(self.nc.sbuf_tensor("sb_" + name, shape, dt))

    def dram_in(self, name, shape, dt=F32):
        return self.nc.dram_tensor(name, list(shape), dt, kind="ExternalInput").ap()

    def dram_out(self, name, shape, dt=F32):
        return self.nc.dram_tensor(name, list(shape), dt, kind="ExternalOutput").ap()

    bank_list = list(range(8))

    def bank(self):
        b = self.bank_list[self.nbank % len(self.bank_list)]
        self.nbank += 1
        return b

    def load(self, dst_ap, src_ap, key, eng="sp", lane=None):
        nc = self.nc
        if lane is None:
            self.uid += 1
            lane = f"ld{self.uid}"
        else:
            self.S.batch_lanes.add(lane)
        h = nc.sync if eng == "sp" else nc.gpsimd
        return self.S.dma(eng, lane, lambda: h.dma_start(out=dst_ap, in_=src_ap), writes=[key])

    def store(self, dst_ap, src_ap, keys, lane=None, batch=True, eng="sp"):
        nc = self.nc
        if lane is None:
            self.uid += 1
            lane = f"st{self.uid}"
        elif batch:
            self.S.batch_lanes.add(lane)
        h = nc.sync if eng == "sp" else nc.gpsimd
        o = self.S.dma(eng, lane, lambda: h.dma_start(out=dst_ap, in_=src_ap), reads=keys)
        self.outs.append(o)
        return o

    def finish(self):
        self.S.emit(final_waits=self.outs)
        self.st.close()
        return self.nc

    def load_slot(self, pieces, pre=None):
        nc = self.nc
        s = self.nslot % self.NSLOT
        self.nslot += 1
        slot = self.slots[s]
        if pre is not None:
            dst = slot[:, :, :].rearrange("p k m -> p (k m)")
            self.S.dma("pool", f"slot{s}",
                       (lambda dst=dst, src=pre: nc.gpsimd.dma_start(out=dst, in_=src, max_dma_last_dim=8192)),
                       writes=[("slot", s, p[4]) for p in pieces])
            return s
        for (W, k0, m0, ncols, off) in pieces:
            src = W[k0:k0 + 2048, m0:m0 + ncols].rearrange("(kc p) m -> p kc m", p=128)
            dst = slot[:, :, off:off + ncols]
            self.S.dma("pool", f"slot{s}",
                       (lambda dst=dst, src=src: nc.gpsimd.dma_start(out=dst, in_=src, max_dma_last_dim=4096)),
                       writes=[("slot", s, off)])
        return s

    def linear(self, W, K, M, rhs_fn, rhs_keys_fn, N, evac, col_groups=None, blocks_fn=None, pre=False):
        nc = self.nc
        if col_groups is None:
            col_groups = [[(m0, 512)] for m0 in range(0, M, 512)]
        nk = K // 2048
        for gi, grp in enumerate(col_groups):
            blocks = blocks_fn(gi) if blocks_fn is not None else [(0, 128), (128, 128), (256, 128), (384, 128)]
            nb = len(blocks)
            bks = [self.bank() for _ in range(nb)]
            for ks in range(nk):
                pieces = []
                off = 0
                for (m0, ncols) in grp:
                    pieces.append((W, ks * 2048, m0, ncols, off))
                    off += ncols
                s = self.load_slot(pieces, pre=(W[gi * nk + ks] if pre else None))
                slot = self.slots[s]
                rkeys = [("slot", s, p[4]) for p in pieces]
                for mb in range(nb):
                    o0, wd = blocks[mb]
                    for kc in range(16):
                        kk = ks * 16 + kc
                        first = (ks == 0 and kc == 0)
                        last = (ks == nk - 1 and kc == 15)
                        self.S.op("pe",
                                  (lambda b=bks[mb], slot=slot, kc=kc, o0=o0, wd=wd, kk=kk, first=first, last=last:
                                   nc.tensor.matmul(self.banks[b][:wd, :N], lhsT=slot[:, kc, o0:o0 + wd],
                                                    rhs=rhs_fn(kk), start=first, stop=last)),
                                  reads=rkeys + list(rhs_keys_fn(kk)), writes=[("ps", bks[mb])])
            evac(gi, bks)

    def rms_rstd(self, src, skey, ncol0, N, scratch, sckey, rstd, rkey, nch=DC):
        nc = self.nc
        b = self.bank()
        for c in range(nch):
            self.S.op("act", (lambda c=c: nc.scalar.activation(out=scratch[:, c, :N], in_=src[:, c, ncol0:ncol0 + N],
                                                               func=AF.Square)),
                      reads=[(skey, c)], writes=[(sckey, c)])
            self.S.op("pe", (lambda c=c: nc.tensor.matmul(self.banks[b][:, :N], lhsT=self.ones[:], rhs=scratch[:, c, :N],
                                                          start=(c == 0), stop=(c == nch - 1))),
                      reads=[(sckey, c), "ones"], writes=[("ps", b)])
        self.S.op("act", lambda: nc.scalar.activation(out=rstd[:, :N], in_=self.banks[b][:, :N], func=AF.Sqrt,
                                                      scale=1.0 / (128 * nch), bias=self.epsc[:, 0:1]),
                  reads=[("ps", b), "epsc"], writes=[rkey])
        self.S.op("dve", lambda: nc.vector.reciprocal(out=rstd[:, :N], in_=rstd[:, :N]), reads=[rkey], writes=[rkey])

    def consts(self):
        nc = self.nc
        self.epsc = self.sb("epsc", [128, 2], F32)
        self.S.op("dve", lambda: nc.vector.memset(self.epsc[:, 0:1], EPS), writes=["epsc"])
        self.S.op("dve", lambda: nc.vector.memset(self.epsc[:, 1:2], LN_EPS), writes=["epsc"])

    def norm_apply(self, src, skey, ncol0, N, gains, gcol0, rstd, rkey, dst, dkey, dcol0, nch=DC, eng="dve"):
        nc = self.nc
        for c in range(nch):
            self.S.op("dve", (lambda c=c: nc.vector.scalar_tensor_tensor(
                out=dst[:, c, dcol0:dcol0 + N], in0=src[:, c, ncol0:ncol0 + N],
                scalar=gains[:, gcol0 + c:gcol0 + c + 1], in1=rstd[:, :N], op0=ALU.mult, op1=ALU.mult)),
                reads=[(skey, c), rkey, "gains"], writes=[(dkey, c)])

    def norm_add(self, src, skey, N, gains, gcol0, rstd, rkey, res, reskey, rcol0, tmp, tkey):
        nc = self.nc
        for c in range(DC):
            self.S.op("dve", (lambda c=c: nc.vector.scalar_tensor_tensor(
                out=tmp[:, c % 2, :N], in0=src[:, c, :N], scalar=gains[:, gcol0 + c:gcol0 + c + 1],
                in1=rstd[:, :N], op0=ALU.mult, op1=ALU.mult)),
                reads=[(skey, c), rkey, "gains"], writes=[(tkey, c % 2)])
            self.S.op("dve", (lambda c=c: nc.vector.tensor_tensor(
                out=res[:, c, rcol0:rcol0 + N], in0=res[:, c, rcol0:rcol0 + N], in1=tmp[:, c % 2, :N], op=ALU.add)),
                reads=[(tkey, c % 2), (reskey, c)], writes=[(reskey, c)])

    def mlp(self, res, reskey, rcol0, N, gains, g_in, g_out, w_up, w_down, xn, xnkey, hbuf, tmpA, rstd, tmp2):
        nc = self.nc
        S = self.S
        self.rms_rstd(res, reskey, rcol0, N, hbuf, "h", rstd, "rstd")
        self.norm_apply(res, reskey, rcol0, N, gains, g_in, rstd, "rstd", xn, xnkey, 0)

        def evac_up(gi, bks):
            for mb in range(4):
                f = gi * 4 + mb
                S.op("act", (lambda b=bks[mb], mb=mb: nc.scalar.activation(out=tmp2[:, mb, :N], in_=self.banks[b][:, :N],
                                                                          func=AF.Relu)),
                     reads=[("ps", bks[mb])], writes=[("tmp2", mb)])
                S.op("dve", (lambda b=bks[mb], mb=mb, f=f: nc.vector.tensor_tensor(
                    out=hbuf[:, f, :N], in0=self.banks[b][:, :N], in1=tmp2[:, mb, :N], op=ALU.mult)),
                    reads=[("ps", bks[mb]), ("tmp2", mb)], writes=[("h", f)])

        self.linear(w_up, D, DFF, lambda kk: xn[:, kk, :N], lambda kk: [(xnkey, kk)], N, evac_up, pre=True)

        def evac_down(gi, bks):
            for mb in range(4):
                c = gi * 4 + mb
                S.op("act", (lambda b=bks[mb], c=c: nc.scalar.copy(out=tmpA[:, c, :N], in_=self.banks[b][:, :N])),
                     reads=[("ps", bks[mb])], writes=[("tmpA", c)])

        self.linear(w_down, DFF, D, lambda kk: hbuf[:, kk, :N], lambda kk: [("h", kk)], N, evac_down, pre=True)
        self.rms_rstd(tmpA, "tmpA", 0, N, xn, xnkey, rstd, "rstd")
        self.norm_add(tmpA, "tmpA", N, gains, g_out, rstd, "rstd", res, reskey, rcol0, tmp2, "tmp2")


def run(prog_nc, in_maps):
    return run_bass_kernel_spmd(prog_nc, in_maps, core_ids=list(range(NCORE))).results


def seg_of(core):
    return (core, NSEG - 1 - core)


def to_fm(a):
    T, F = a.shape
    return np.ascontiguousarray(a.T.reshape(F // 128, 128, T))


def from_fm(a):
    C, P, T = a.shape
    return np.ascontiguousarray(a.reshape(C * P, T).T)


def col_layout(v):
    return np.ascontiguousarray(v.reshape(-1, 128).T)


def build_layer0():
    P = Prog()
    nc, S = P.nc, P.S
    N = SEG
    NH = N + HALO
    xin_d = P.dram_in("xin", [2, DC, 128, N])
    xh_d = P.dram_in("xh", [DC, 128, 2 * HALO])
    hmask_d = P.dram_in("hmask", [128, 2 * HALO])
    gains_d = P.dram_in("gains", [128, 4 * DC])
    vecs_d = P.dram_in("vecs", [128, 6 * DC])
    wdw_d = P.dram_in("wdw", [128, DC, 31])
    ident_d = P.dram_in("ident", [128, 128])
    w_in = P.dram_in("w_in", [8, 128, 8192])
    w_out = P.dram_in("w_out", [4, 128, 8192])
    w_up = P.dram_in("w_up", [16, 128, 8192])
    w_down = P.dram_in("w_down", [16, 128, 8192])
    out_d = P.dram_out("out", [2, DC, 128, N])

    P.consts()
    xin = P.sb("xin", [128, DC, N], F32)
    xh = P.sb("xh", [128, DC, 2 * HALO], F32)
    xn = P.sb("xn", [128, DC, N], BF16)
    tmpA = P.sb("tmpA", [128, DC, N], F32)
    big = P.sb("big", [128, 64 * N], BF16)
    hbuf = big[:, :].rearrange("p (c n) -> p c n", n=N)
    hglu = big[:, 32 * N:32 * N + DC * NH].rearrange("p (c n) -> p c n", n=NH)
    diag = big[:, 50 * N:50 * N + 31 * 128].rearrange("p (k m) -> p k m", m=128)

    def hglu_alias(c):
        lo = (32 * N + c * NH) // N
        hi = (32 * N + (c + 1) * NH - 1) // N
        return [("h", f) for f in range(lo, hi + 1)]
    diag_alias = [("h", f) for f in range(50, 58)]
    rstd = P.sb("rstd", [128, N], F32)
    mean = P.sb("mean", [128, N], F32)
    tmp2 = P.sb("tmp2", [128, 4, N], F32)
    gains = P.sb("gains_sb", [128, 4 * DC], F32)
    vecs = P.sb("vecs_sb", [128, 6 * DC], F32)
    wdw = P.sb("wdw_sb", [128, DC, 31], F32)
    ident = P.sb("ident_sb", [128, 128], F32)
    hmask = P.sb("hmask_sb", [128, 2 * HALO], F32)
    hgh = P.sb("hgh", [128, DC, 2 * HALO], BF16)

    P.load(gains[:], gains_d[:, :], "gains")
    P.load(vecs[:], vecs_d[:, :], "vecs")
    P.load(wdw[:], wdw_d[:, :, :], "wdw")
    P.load(ident[:], ident_d[:, :], "ident")
    P.load(hmask[:], hmask_d[:, :], "hmask")
    for c in range(DC):
        P.load(xh[:, c, :], xh_d[c], ("xh", c), lane="xh")

    groups = [[(256 * g, 256), (2048 + 256 * g, 256)] for g in range(8)]

    def inproj(src, skey, n, dst, dkey, dcol0, masked):
        P.rms_rstd(src, skey, 0, n, hbuf, "h", rstd, "rstd")
        P.norm_apply(src, skey, 0, n, gains, 0, rstd, "rstd", xn, "xn", 0)

        def evac_in(gi, bks):
            for j in range(2):
                c = gi * 2 + j
                ba, bg = bks[j], bks[2 + j]
                S.op("act", (lambda bg=bg, c=c, j=j: nc.scalar.activation(
                    out=tmp2[:, j, :n], in_=P.banks[bg][:, :n], func=AF.Sigmoid,
                    bias=vecs[:, DC + c:DC + c + 1])),
                    reads=[("ps", bg), "vecs"], writes=[("tmp2", j)])
                S.op("dve", (lambda ba=ba, c=c, j=j: nc.vector.scalar_tensor_tensor(
                    out=dst[:, c, dcol0:dcol0 + n], in0=P.banks[ba][:, :n], scalar=vecs[:, c:c + 1],
                    in1=tmp2[:, j, :n], op0=ALU.add, op1=ALU.mult)),
                    reads=[("ps", ba), ("tmp2", j), "vecs"], writes=[(dkey, c)] + (hglu_alias(c) if dkey == "hglu" else []))
                if masked:
                    S.op("dve", (lambda c=c: nc.vector.tensor_tensor(
                        out=dst[:, c, 0:n], in0=dst[:, c, 0:n], in1=hmask[:], op=ALU.mult)),
                        reads=[(dkey, c), "hmask"], writes=[(dkey, c)])
        P.linear(w_in, D, 2 * D, (lambda kk: xn[:, kk, :n]), lambda kk: [("xn", kk)], n,
                 evac_in, col_groups=groups, pre=True)

    inproj(xh, "xh", 2 * HALO, hgh, "hgh", 0, True)

    for t in range(2):
        for c in range(DC):
            P.load(xin[:, c, :], xin_d[t, c], ("xin", c), lane=f"xin{t}")
        for c in range(DC):
            S.op("dve", (lambda c=c, t=t: nc.vector.tensor_copy(out=hglu[:, c, 0:HALO],
                                                                 in_=hgh[:, c, t * HALO:(t + 1) * HALO])),
                 reads=[("hgh", c)], writes=[("hglu", c)] + hglu_alias(c))
        inproj(xin, "xin", N, hglu, "hglu", HALO, False)
        for c in range(DC):
            for k in range(31):
                S.op("dve", (lambda c=c, k=k: nc.vector.tensor_scalar(
                    out=diag[:, k, :], in0=ident[:], scalar1=wdw[:, c, k:k + 1], scalar2=None, op0=ALU.mult)),
                    reads=["ident", "wdw"], writes=[("diag", k)] + (diag_alias if k == 0 else []))
            b = P.bank()
            for k in range(31):
                S.op("pe", (lambda c=c, k=k, b=b: nc.tensor.matmul(
                    P.banks[b][:, :N], lhsT=diag[:, k, :], rhs=hglu[:, c, 2 + k:2 + k + N],
                    start=(k == 0), stop=(k == 30))),
                    reads=[("diag", k), ("hglu", c)] + hglu_alias(c) + diag_alias, writes=[("ps", b)])
            S.op("act", (lambda c=c, b=b: nc.scalar.activation(
                out=tmpA[:, c, :], in_=P.banks[b][:, :N], func=AF.Identity, bias=vecs[:, 2 * DC + c:2 * DC + c + 1])),
                reads=[("ps", b), "vecs"], writes=[("tmpA", c)])
        bs, bq = P.bank(), P.bank()
        for c in range(DC):
            S.op("act", (lambda c=c: nc.scalar.copy(out=hbuf[:, c, :N], in_=tmpA[:, c, :])),
                 reads=[("tmpA", c)], writes=[("h", c)])
            S.op("act", (lambda c=c: nc.scalar.activation(out=hbuf[:, 16 + c, :N], in_=tmpA[:, c, :], func=AF.Square)),
                 reads=[("tmpA", c)], writes=[("h", 16 + c)])
            S.op("pe", (lambda c=c, bs=bs: nc.tensor.matmul(P.banks[bs][:, :N], lhsT=P.ones[:], rhs=hbuf[:, c, :N],
                                                     start=(c == 0), stop=(c == DC - 1))),
                 reads=[("h", c), "ones"], writes=[("ps", bs)])
            S.op("pe", (lambda c=c, bq=bq: nc.tensor.matmul(P.banks[bq][:, :N], lhsT=P.ones[:], rhs=hbuf[:, 16 + c, :N],
                                                     start=(c == 0), stop=(c == DC - 1))),
                 reads=[("h", 16 + c), "ones"], writes=[("ps", bq)])
        S.op("act", lambda bs=bs: nc.scalar.activation(out=mean[:], in_=P.banks[bs][:, :N], func=AF.Copy, scale=1.0 / D),
             reads=[("ps", bs)], writes=["mean"])
        S.op("dve", lambda: nc.vector.tensor_tensor(out=tmp2[:, 0, :], in0=mean[:], in1=mean[:], op=ALU.mult),
             reads=["mean"], writes=[("tmp2", 0)])
        S.op("dve", lambda bq=bq: nc.vector.scalar_tensor_tensor(out=tmp2[:, 1, :], in0=P.banks[bq][:, :N], scalar=1.0 / D,
                                                           in1=tmp2[:, 0, :], op0=ALU.mult, op1=ALU.subtract),
             reads=[("ps", bq), ("tmp2", 0)], writes=[("tmp2", 1)])
        S.op("act", lambda: nc.scalar.activation(out=rstd[:, :N], in_=tmp2[:, 1, :], func=AF.Sqrt,
                                                 bias=P.epsc[:, 1:2]),
             reads=[("tmp2", 1), "epsc"], writes=["rstd"])
        S.op("dve", lambda: nc.vector.reciprocal(out=rstd[:, :N], in_=rstd[:, :N]), reads=["rstd"], writes=["rstd"])
        for c in range(DC):
            j = 2 + (c % 2)
            S.op("dve", (lambda c=c, j=j: nc.vector.tensor_tensor(out=tmp2[:, j, :], in0=tmpA[:, c, :], in1=mean[:],
                                                                  op=ALU.subtract)),
                 reads=[("tmpA", c), "mean"], writes=[("tmp2", j)])
            S.op("dve", (lambda c=c, j=j: nc.vector.tensor_tensor(out=tmp2[:, j, :], in0=tmp2[:, j, :], in1=rstd[:, :N],
                                                                  op=ALU.mult)),
                 reads=[("tmp2", j), "rstd"], writes=[("tmp2", j)])
            S.op("act", (lambda c=c, j=j: nc.scalar.activation(
                out=xn[:, c, :N], in_=tmp2[:, j, :], func=AF.Silu,
                scale=vecs[:, 3 * DC + c:3 * DC + c + 1], bias=vecs[:, 4 * DC + c:4 * DC + c + 1])),
                reads=[("tmp2", j), "vecs"], writes=[("xn", c)])

        def evac_out(gi, bks):
            for mb in range(4):
                c = gi * 4 + mb
                S.op("act", (lambda b=bks[mb], c=c: nc.scalar.copy(out=tmpA[:, c, :], in_=P.banks[b][:, :N])),
                     reads=[("ps", bks[mb])], writes=[("tmpA", c)])
        P.linear(w_out, D, D, lambda kk: xn[:, kk, :N], lambda kk: [("xn", kk)], N, evac_out, pre=True)
        P.rms_rstd(tmpA, "tmpA", 0, N, hbuf, "h", rstd, "rstd")
        P.norm_add(tmpA, "tmpA", N, gains, DC, rstd, "rstd", xin, "xin", 0, tmp2, "tmp2")
        P.mlp(xin, "xin", 0, N, gains, 2 * DC, 3 * DC, w_up, w_down, xn, "xn", hbuf, tmpA, rstd, tmp2)
        for c in range(DC):
            P.store(out_d[t, c], xin[:, c, :], [("xin", c)], lane=f"out{t}")
    return P.finish()


POOL_W = (2, 4, 8, 16)


def build_layer1():
    P = Prog()
    nc, S = P.nc, P.S
    N = SEG
    HALO = 16
    NH = N + HALO
    xin_d = P.dram_in("xin", [2, DC, 128, N])
    xh_d = P.dram_in("xh", [DC, 128, 2 * HALO])
    rdiv_d = P.dram_in("rdiv", [2, 128, 4, N])
    gains_d = P.dram_in("gains", [128, 4 * DC])
    pscale_d = P.dram_in("pscale", [128, DC])
    w_pool = P.dram_in("w_pool", [D, 512])
    w_up = P.dram_in("w_up", [16, 128, 8192])
    w_down = P.dram_in("w_down", [16, 128, 8192])
    out_d = P.dram_out("out", [2, DC, 128, N])

    P.consts()
    xin = P.sb("xin", [128, DC, N], F32)
    xh = P.sb("xh", [128, DC, 2 * HALO], F32)
    hh = P.sb("hh", [128, DC, 2 * HALO], F32)
    xn = P.sb("xn", [128, DC, N], BF16)
    tmpA = P.sb("tmpA", [128, DC, NH], F32)
    hbuf = P.sb("hbuf", [128, 64, N], BF16)
    rstd = P.sb("rstd", [128, N], F32)
    tmp2 = P.sb("tmp2", [128, 4, N], F32)
    sbuf2 = P.sb("sbuf2", [128, 2, NH], F32)
    rdiv = P.sb("rdiv", [128, 4, N], F32)
    gains = P.sb("gains_sb", [128, 4 * DC], F32)
    pscale = P.sb("pscale_sb", [128, DC], F32)

    P.load(gains[:], gains_d[:, :], "gains")
    P.load(pscale[:], pscale_d[:, :], "pscale")
    for c in range(DC):
        P.load(xh[:, c, :], xh_d[c], ("xh", c), lane="xh")
    P.rms_rstd(xh, "xh", 0, 2 * HALO, hbuf, "h", rstd, "rstd")
    for c in range(DC):
        S.op("dve", (lambda c=c: nc.vector.scalar_tensor_tensor(
            out=hh[:, c, :], in0=xh[:, c, :], scalar=gains[:, c:c + 1], in1=rstd[:, :2 * HALO],
            op0=ALU.mult, op1=ALU.mult)), reads=[("xh", c), "rstd", "gains"], writes=[("hh", c)])

    for t in range(2):
        P.load(rdiv[:], rdiv_d[t], "rdiv")
        for c in range(DC):
            P.load(xin[:, c, :], xin_d[t, c], ("xin", c), lane=f"xin{t}")
        P.rms_rstd(xin, "xin", 0, N, hbuf, "h", rstd, "rstd")
        hf = tmpA
        for c in range(DC):
            S.op("dve", (lambda c=c, t=t: nc.vector.tensor_copy(out=hf[:, c, 0:HALO], in_=hh[:, c, t * HALO:(t + 1) * HALO])),
                 reads=[("hh", c)], writes=[("tmpA", c)])
            S.op("dve", (lambda c=c: nc.vector.scalar_tensor_tensor(
                out=hf[:, c, HALO:NH], in0=xin[:, c, :], scalar=gains[:, c:c + 1], in1=rstd[:, :N],
                op0=ALU.mult, op1=ALU.mult)), reads=[("xin", c), "rstd", "gains", ("tmpA", c)], writes=[("tmpA", c)])
        for c in range(DC):
            g = c // 4
            nstep = g + 1
            cur = hf[:, c, :]
            curkey = ("tmpA", c)
            for si in range(nstep):
                sh = 1 << si
                lo = 2 * sh - 1
                dst = sbuf2[:, si % 2, :]
                dkey = ("sbuf2", si % 2)
                S.op("dve", (lambda cur=cur, dst=dst, sh=sh, lo=lo: nc.vector.tensor_tensor(
                    out=dst[:, lo:NH], in0=cur[:, lo:NH], in1=cur[:, lo - sh:NH - sh], op=ALU.add)),
                    reads=[curkey], writes=[dkey])
                cur, curkey = dst, dkey
            S.op("dve", (lambda cur=cur, g=g: nc.vector.tensor_tensor(
                out=tmp2[:, 0, :], in0=cur[:, HALO:NH], in1=rdiv[:, g, :], op=ALU.mult)),
                reads=[curkey, "rdiv"], writes=[("tmp2", 0)])
            S.op("dve", (lambda c=c: nc.vector.tensor_tensor(
                out=xn[:, c, :], in0=tmp2[:, 0, :], in1=hf[:, c, HALO:NH], op=ALU.subtract)),
                reads=[("tmp2", 0), ("tmpA", c)], writes=[("xn", c)])
        s = P.load_slot([(w_pool, 0, 0, 512, 0)])
        slot = P.slots[s]
        for g in range(4):
            bks = [P.bank() for _ in range(4)]
            for mb in range(4):
                for kc in range(4):
                    S.op("pe", (lambda g=g, mb=mb, kc=kc, b=bks[mb], slot=slot: nc.tensor.matmul(
                        P.banks[b][:, :N], lhsT=slot[:, g * 4 + kc, mb * 128:(mb + 1) * 128], rhs=xn[:, g * 4 + kc, :],
                        start=(kc == 0), stop=(kc == 3))),
                        reads=[("slot", s, 0), ("xn", g * 4 + kc)], writes=[("ps", bks[mb])])
            for mb in range(4):
                c = g * 4 + mb
                S.op("act", (lambda b=bks[mb], c=c: nc.scalar.activation(
                    out=tmpA[:, c, :N], in_=P.banks[b][:, :N], func=AF.Copy, scale=pscale[:, c:c + 1])),
                    reads=[("ps", bks[mb]), "pscale"], writes=[("tmpA", c)])
        P.rms_rstd(tmpA, "tmpA", 0, N, hbuf, "h", rstd, "rstd")
        P.norm_add(tmpA, "tmpA", N, gains, DC, rstd, "rstd", xin, "xin", 0, tmp2, "tmp2")
        P.mlp(xin, "xin", 0, N, gains, 2 * DC, 3 * DC, w_up, w_down, xn, "xn", hbuf, tmpA, rstd, tmp2)
        for c in range(DC):
            P.store(out_d[t, c], xin[:, c, :], [("xin", c)], lane=f"out{t}")
    return P.finish()


QW, KVW, QIW, IDXD, IDXH = 2048, 512, 1024, 64, 16
ROPE_THETA = 500000.0


def build_l2a():
    P = Prog()
    nc, S = P.nc, P.S
    N = SEG
    xin_d = P.dram_in("xin", [2, DC, 128, N])
    rope_d = P.dram_in("rope", [2, 128, 4, N])
    rot_d = P.dram_in("rot", [128, 2, 128])
    gains_d = P.dram_in("gains", [128, 4 * DC])
    w_in = P.dram_in("w_in", [D, 4176])
    q_d = P.dram_out("q", [2, 16, 128, N], BF16)
    k_d = P.dram_out("k", [2, 4, 128, N], BF16)
    v_d = P.dram_out("v", [2, 4, 128, N], BF16)
    qi_d = P.dram_out("qi", [2, 8, 128, N], BF16)
    ki_d = P.dram_out("ki", [2, 64, N], BF16)
    wi_d = P.dram_out("wi", [2, 16, N], F32)

    P.consts()
    xin = P.sb("xin", [128, DC, N], F32)
    xn = P.sb("xn", [128, DC, N], BF16)
    scr = P.sb("scr", [128, DC, N], BF16)
    rstd = P.sb("rstd", [128, N], F32)
    outb = P.sb("outb", [128, 34, N], BF16)
    wib = P.sb("wib", [128, N], F32)
    qraw = P.sb("qraw", [128, 8, N], BF16)
    tt = P.sb("tt", [128, 4, N], F32)
    rope = P.sb("rope_sb", [128, 4, N], F32)
    rotf = P.sb("rotf", [128, 2, 128], F32)
    rot = P.sb("rot_sb", [128, 2, 128], BF16)
    gains = P.sb("gains_sb", [128, 4 * DC], F32)
    P.load(gains[:], gains_d[:, :], "gains")
    P.load(rotf[:], rot_d[:, :, :], "rotf")
    S.op("dve", lambda: nc.vector.tensor_copy(out=rot[:], in_=rotf[:]), reads=["rotf"], writes=["rot"])

    groups = [[(512 * g, 512)] for g in range(8)] + [[(4096, 80)]]

    def blocks_fn(gi):
        return [(0, 80)] if gi == 8 else [(0, 128), (128, 128), (256, 128), (384, 128)]

    for t in range(2):
        P.load(rope[:], rope_d[t], "rope")
        for c in range(DC):
            P.load(xin[:, c, :], xin_d[t, c], ("xin", c), lane=f"xin{t}")
        P.rms_rstd(xin, "xin", 0, N, scr, "scr", rstd, "rstd")
        P.norm_apply(xin, "xin", 0, N, gains, 0, rstd, "rstd", xn, "xn", 0)
        deferred = []
        cnt = [0]

        def rope_chunk(b, oc, np_, ti, t=t):
            j = cnt[0] % 8
            cnt[0] += 1
            S.op("act", (lambda: nc.scalar.copy(out=qraw[:np_, j, :], in_=P.banks[b][:np_, :N])),
                 reads=[("ps", b)], writes=[("qraw", j)])

            def later():
                rb = P.bank()
                S.op("pe", (lambda: nc.tensor.matmul(P.banks[rb][:np_, :N], lhsT=rot[:np_, ti, :np_], rhs=qraw[:np_, j, :],
                                                     start=True, stop=True)),
                     reads=[("qraw", j), "rot"], writes=[("ps", rb)])
                a = (2 * j) % 4
                S.op("dve", (lambda: nc.vector.tensor_tensor(out=tt[:np_, a, :], in0=qraw[:np_, j, :],
                                                             in1=rope[:np_, 2 * ti, :], op=ALU.mult)),
                     reads=[("qraw", j), "rope"], writes=[("tt", a)])
                S.op("dve", (lambda: nc.vector.tensor_tensor(out=tt[:np_, a + 1, :], in0=P.banks[rb][:np_, :N],
                                                             in1=rope[:np_, 2 * ti + 1, :], op=ALU.mult)),
                     reads=[("ps", rb), "rope"], writes=[("tt", a + 1)])
                S.op("dve", (lambda: nc.vector.tensor_tensor(out=outb[:np_, oc, :], in0=tt[:np_, a, :],
                                                             in1=tt[:np_, a + 1, :], op=ALU.add)),
                     reads=[("tt", a), ("tt", a + 1)], writes=[("outb", oc)])
            deferred.append(later)

        def evac(gi, bks, t=t):
            prev = list(deferred)
            del deferred[:]
            if gi < 5:
                for mb in range(4):
                    rope_chunk(bks[mb], gi * 4 + mb, 128, 0)
            elif gi == 5:
                for mb in range(4):
                    S.op("act", (lambda b=bks[mb], oc=20 + mb: nc.scalar.copy(out=outb[:, oc, :], in_=P.banks[b][:, :N])),
                         reads=[("ps", bks[mb])], writes=[("outb", 20 + mb)])
            elif gi < 8:
                for mb in range(4):
                    rope_chunk(bks[mb], 24 + (gi - 6) * 4 + mb, 128, 1)
            else:
                b = bks[0]
                S.op("act", (lambda b=b: nc.scalar.activation(out=wib[64:80, :], in_=P.banks[b][64:80, :N], func=AF.Copy,
                                                              scale=1.0 / 32.0)),
                     reads=[("ps", b)], writes=["wib"])
                rope_chunk(b, 32, 64, 1)
            for f in prev:
                f()

        P.linear(w_in, D, 4176, lambda kk: xn[:, kk, :N], lambda kk: [("xn", kk)], N, evac,
                 col_groups=groups, blocks_fn=blocks_fn)
        for f in deferred:
            f()
        del deferred[:]
        for c in range(16):
            P.store(q_d[t, c], outb[:, c, :], [("outb", c)], lane=f"out{t}")
        for c in range(4):
            P.store(k_d[t, c], outb[:, 16 + c, :], [("outb", 16 + c)], lane=f"out{t}")
            P.store(v_d[t, c], outb[:, 20 + c, :], [("outb", 20 + c)], lane=f"out{t}")
        for c in range(8):
            P.store(qi_d[t, c], outb[:, 24 + c, :], [("outb", 24 + c)], lane=f"out{t}")
        P.store(ki_d[t], outb[0:64, 32, :], [("outb", 32)], lane=f"out{t}")
        P.store(wi_d[t], wib[64:80, :], ["wib"], lane=f"out{t}")
    return P.finish()


def rope_tables(seg):
    pos = np.arange(seg * SEG, (seg + 1) * SEG, dtype=np.float64)
    tab = np.zeros((128, 4, SEG), np.float32)
    tab[:, 0, :] = 1.0
    tab[:, 2, :] = 1.0
    invq = ROPE_THETA ** (-2.0 * np.arange(16) / 32.0)
    invi = ROPE_THETA ** (-2.0 * np.arange(8) / 16.0)
    angq = (pos.astype(np.float32)[None, :] * invq.astype(np.float32)[:, None]).astype(np.float32)
    angi = (pos.astype(np.float32)[None, :] * invi.astype(np.float32)[:, None]).astype(np.float32)
    for m in range(32):
        tab[m, 0, :] = np.cos(angq[m % 16])
        tab[m, 1, :] = np.sin(angq[m % 16])
    for b in (0, 64):
        for jj in range(16):
            tab[b + jj, 2, :] = np.cos(angi[jj % 8])
            tab[b + jj, 3, :] = np.sin(angi[jj % 8])
    return tab


def rot_mats():
    r = np.zeros((128, 2, 128), np.float32)
    for m in range(16):
        r[m + 16, 0, m] = -1.0
        r[m, 0, m + 16] = 1.0
    for b in (0, 64):
        for j in range(8):
            r[b + j + 8, 1, b + j] = -1.0
            r[b + j, 1, b + j + 8] = 1.0
    return r


def run_l2a(x_fm, inp):
    nc = get_nc("l2a", build_l2a)
    common = {"gains": gains_layout(inp["norm_gains"][2]), "rot": rot_mats(), "w_in": inp["attn_w_in"][0]}
    maps = []
    for core in range(NCORE):
        m = dict(common)
        m.update({"xin": seg_fm(x_fm, core), "rope": np.stack([rope_tables(s) for s in seg_of(core)])})
        maps.append(m)
    outs = run(nc, maps)
    full = {}
    for name in ("q", "k", "v", "qi"):
        full[name] = gather_fm(outs, name)
    for name in ("ki", "wi"):
        a0 = outs[0][name]
        f = np.empty((a0.shape[1], L), a0.dtype)
        for core in range(NCORE):
            for i, s in enumerate(seg_of(core)):
                f[:, s * SEG:(s + 1) * SEG] = outs[core][name][i]
        full[name] = f
    return full


NQB = 8
BIS_ITERS = 14
TOPK = 256


def build_l2b(nqb=NQB, stop_after=None):
    P = Prog()
    P.bank_list = [4, 5, 6, 7]
    nc, S = P.nc, P.S
    q_d = P.dram_in("q", [NQB, 128, 16, 128], BF16)
    qi_d = P.dram_in("qi", [NQB, 128, 8, 128], BF16)
    wi_d = P.dram_in("wi", [NQB, 128, 16])
    ki_d = P.dram_in("ki", [128, L], BF16)
    k_d = P.dram_in("k", [4, 128, L], BF16)
    v_d = P.dram_in("v", [4, 128, 64, 128], BF16)
    cm_d = P.dram_in("cmask", [128, 1024])
    id_d = P.dram_in("ident", [128, 128])
    o_d = P.dram_out("o", [NQB, 4, 128, 512], BF16)

    Ibuf = P.sb("I", [128, L], F32)
    Rb = P.sb("R", [128, 2, 512], F32)
    Mb = P.sb("Mb", [128, L], BF16)
    MbT = P.sb("MbT", [128, 64, 128], BF16)
    ki = P.sb("ki_sb", [128, L], BF16)
    kTb = P.sb("kTb", [128, 2, L], BF16)
    Vb = P.sb("Vb", [128, 2, 64, 128], BF16)
    qsb = P.sb("q_sb", [128, 2, 16, 128], BF16)
    qisb = P.sb("qi_sb", [128, 2, 8, 128], BF16)
    PT = P.sb("PT", [128, 2, 512], BF16)
    rden = P.sb("rden", [128, 512], F32)
    ob = P.sb("ob", [128, 2, 512], BF16)
    cmask = P.sb("cmask_sb", [128, 1024], F32)
    identf = P.sb("identf", [128, 128], F32)
    identb = P.sb("identb", [128, 128], BF16)
    wis = P.sb("wis", [128, 2, 16], F32)
    wabs = P.sb("wabs", [128, 2, 16], F32)
    wsgn = P.sb("wsgn", [128, 2, 16], F32)
    sm = P.sb("sm", [128, 16], F32)

    P.load(cmask[:], cm_d[:, :], "cmask")
    P.load(identf[:], id_d[:, :], "identf")
    S.op("dve", lambda: nc.vector.tensor_copy(out=identb[:], in_=identf[:]), reads=["identf"], writes=["identb"])
    for g in range(4):
        P.load(ki[:, g * 2048:(g + 1) * 2048], ki_d[:, g * 2048:(g + 1) * 2048], ("ki", g), lane="ki")
    BD = [0, 1]
    BO = [2, 3]
    acc_i = 0
    for i in range(nqb):
        nk = 1024 * (i + 1)
        ng = nk // 512
        nkb = nk // 128
        qs = i % 2
        P.load(qsb[:, qs], q_d[i], ("q", qs), lane=f"qin{i}")
        P.load(qisb[:, qs], qi_d[i], ("qi", qs), lane=f"qin{i}")
        P.load(wis[:, qs, :], wi_d[i], ("wi", qs), lane=f"qin{i}")
        S.op("dve", (lambda qs=qs: nc.vector.tensor_scalar(out=wabs[:, qs, :], in0=wis[:, qs, :], scalar1=-1.0, scalar2=None,
                                                           op0=ALU.mult)),
             reads=[("wi", qs)], writes=[("wabs", qs)])
        S.op("dve", (lambda qs=qs: nc.vector.tensor_tensor(out=wabs[:, qs, :], in0=wabs[:, qs, :], in1=wis[:, qs, :],
                                                           op=ALU.max)),
             reads=[("wi", qs), ("wabs", qs)], writes=[("wabs", qs)])
        S.op("dve", (lambda qs=qs: nc.vector.tensor_scalar(out=wsgn[:, qs, :], in0=wis[:, qs, :], scalar1=0.0, scalar2=2.0,
                                                           op0=ALU.is_ge, op1=ALU.mult)),
             reads=[("wi", qs)], writes=[("wsgn", qs)])
        S.op("dve", (lambda qs=qs: nc.vector.tensor_scalar(out=wsgn[:, qs, :], in0=wsgn[:, qs, :], scalar1=-1.0,
                                                           scalar2=None, op0=ALU.add)),
             reads=[("wsgn", qs)], writes=[("wsgn", qs)])
        for G in range(ng):
            for h in range(16):
                b = P.bank()
                p0 = 64 * (h % 2)
                ch = h // 2
                S.op("pe", (lambda b=b, p0=p0, ch=ch, G=G, qs=qs: nc.tensor.matmul(
                    P.banks[b][:, :512], lhsT=qisb[p0:p0 + 64, qs, ch, :], rhs=ki[p0:p0 + 64, G * 512:(G + 1) * 512],
                    start=True, stop=True)),
                    reads=[("qi", qs), ("ki", G // 4)], writes=[("ps", b)])
                r = h % 2
                S.op("act", (lambda b=b, r=r, h=h, qs=qs: nc.scalar.activation(
                    out=Rb[:, r, :], in_=P.banks[b][:, :512], func=AF.Relu, scale=wabs[:, qs, h:h + 1])),
                    reads=[("ps", b), ("wabs", qs)], writes=[("R", r)])
                if h == 0:
                    S.op("dve", (lambda r=r, h=h, G=G, qs=qs: nc.vector.tensor_scalar(
                        out=Ibuf[:, G * 512:(G + 1) * 512], in0=Rb[:, r, :], scalar1=wsgn[:, qs, h:h + 1], scalar2=None,
                        op0=ALU.mult)),
                        reads=[("R", r), ("wsgn", qs)], writes=[("I", G)])
                else:
                    S.op("dve", (lambda r=r, h=h, G=G, qs=qs: nc.vector.scalar_tensor_tensor(
                        out=Ibuf[:, G * 512:(G + 1) * 512], in0=Rb[:, r, :], scalar=wsgn[:, qs, h:h + 1],
                        in1=Ibuf[:, G * 512:(G + 1) * 512], op0=ALU.mult, op1=ALU.add)),
                        reads=[("R", r), ("wsgn", qs), ("I", G)], writes=[("I", G)])
        Ikeys = [("I", G) for G in range(ng)]
        if stop_after == 'A':
            continue
        S.op("dve", (lambda nk=nk: nc.vector.tensor_reduce(out=sm[:, 0:1], in_=Ibuf[:, :nk], axis=AX.X, op=ALU.min)),
             reads=Ikeys, writes=[("sm", 0)])
        for G in (ng - 2, ng - 1):
            o = (G - (ng - 2)) * 512
            S.op("dve", (lambda G=G, o=o: nc.vector.tensor_tensor(
                out=Ibuf[:, G * 512:(G + 1) * 512], in0=Ibuf[:, G * 512:(G + 1) * 512], in1=cmask[:, o:o + 512], op=ALU.add)),
                reads=[("I", G), "cmask", ("sm", 0)], writes=[("I", G)])
        S.op("dve", (lambda nk=nk: nc.vector.tensor_reduce(out=sm[:, 1:2], in_=Ibuf[:, :nk], axis=AX.X, op=ALU.max)),
             reads=Ikeys, writes=[("sm", 1)])
        S.op("dve", lambda: nc.vector.tensor_tensor(out=sm[:, 1:2], in0=sm[:, 1:2], in1=sm[:, 0:1], op=ALU.subtract),
             reads=[("sm", 0), ("sm", 1)], writes=[("sm", 1)])
        for it in range(BIS_ITERS):
            S.op("dve", lambda: nc.vector.tensor_scalar(out=sm[:, 1:2], in0=sm[:, 1:2], scalar1=0.5, scalar2=None,
                                                        op0=ALU.mult),
                 reads=[("sm", 1)], writes=[("sm", 1)])
            S.op("dve", lambda: nc.vector.tensor_tensor(out=sm[:, 2:3], in0=sm[:, 0:1], in1=sm[:, 1:2], op=ALU.add),
                 reads=[("sm", 0), ("sm", 1)], writes=[("sm", 2)])
            S.op("dve", (lambda nk=nk: nc.vector.tensor_scalar(
                out=Mb[:, :nk], in0=Ibuf[:, :nk], scalar1=sm[:, 2:3], scalar2=0.0, op0=ALU.is_ge, op1=ALU.add,
                accum_out=sm[:, 3:4])),
                reads=Ikeys + [("sm", 2)], writes=[("sm", 3), "Mb"])
            S.op("dve", lambda: nc.vector.tensor_scalar(out=sm[:, 4:5], in0=sm[:, 3:4], scalar1=float(TOPK) - 0.5,
                                                        scalar2=None, op0=ALU.is_ge),
                 reads=[("sm", 3)], writes=[("sm", 4)])
            S.op("dve", lambda: nc.vector.tensor_tensor(out=sm[:, 4:5], in0=sm[:, 4:5], in1=sm[:, 1:2], op=ALU.mult),
                 reads=[("sm", 4), ("sm", 1)], writes=[("sm", 4)])
            S.op("dve", lambda: nc.vector.tensor_tensor(out=sm[:, 0:1], in0=sm[:, 0:1], in1=sm[:, 4:5], op=ALU.add),
                 reads=[("sm", 0), ("sm", 4)], writes=[("sm", 0)])
        if stop_after == 'B':
            continue
        S.op("dve", (lambda nk=nk: nc.vector.tensor_scalar(
            out=Mb[:, :nk], in0=Ibuf[:, :nk], scalar1=sm[:, 0:1], scalar2=-1.0e5, op0=ALU.is_lt, op1=ALU.mult)),
            reads=Ikeys + [("sm", 0)], writes=["Mb"])
        for g4 in range(nkb // 4):
            b = P.bank()
            for j in range(4):
                kb = g4 * 4 + j
                S.op("pe", (lambda b=b, j=j, kb=kb: nc.tensor.matmul(
                    P.banks[b][:, j * 128:(j + 1) * 128], lhsT=Mb[:, kb * 128:(kb + 1) * 128], rhs=identb[:],
                    start=True, stop=True)),
                    reads=["Mb", "identb"], writes=[("ps", b)])
            S.op("act", (lambda b=b, g4=g4: nc.scalar.copy(
                out=MbT[:, g4 * 4:(g4 + 1) * 4, :], in_=P.banks[b][:, :512].rearrange("p (j q) -> p j q", q=128))),
                reads=[("ps", b)], writes=[("MbT", g4)])
        if stop_after == 'C':
            continue
        for kv in range(4):
            a = acc_i % 2
            acc_i += 1
            P.load(kTb[:, a, :nk], k_d[kv, :, :nk], ("kT", a))
            P.load(Vb[:, a, :nkb, :], v_d[kv, :, :nkb, :], ("V", a))
            bd, bo = BD[a], BO[a]
            pend_pv = None
            for kb in range(nkb):
                b = P.bank()
                pt = kb % 2
                S.op("pe", (lambda b=b, kb=kb, a=a, kv=kv, qs=qs: nc.tensor.matmul(
                    P.banks[b][:, :512], lhsT=kTb[:, a, kb * 128:(kb + 1) * 128], rhs=qsb[:, qs, 4 * kv:4 * kv + 4, :],
                    start=True, stop=False)),
                    reads=[("kT", a), ("q", qs)], writes=[("ps", b)])
                for hh in range(4):
                    S.op("pe", (lambda b=b, kb=kb, hh=hh: nc.tensor.matmul(
                        P.banks[b][:, hh * 128:(hh + 1) * 128], lhsT=identb[:], rhs=MbT[:, kb, :],
                        start=False, stop=(hh == 3))),
                        reads=[("MbT", kb // 4), "identb"], writes=[("ps", b)])
                if stop_after == 'D1':
                    continue
                S.op("act", (lambda b=b, pt=pt: nc.scalar.activation(out=PT[:, pt, :], in_=P.banks[b][:, :512], func=AF.Exp,
                                                                     scale=128.0 ** -0.5)),
                     reads=[("ps", b)], writes=[("PT", pt)])
                if stop_after == 'D2':
                    continue
                if pend_pv is not None:
                    pend_pv()

                def pv(pt=pt, bd=bd, bo=bo, kb=kb, nkb=nkb, a=a):
                    S.op("pe", (lambda: nc.tensor.matmul(
                        P.banks[bd][:, :512], lhsT=P.ones[:], rhs=PT[:, pt, :], start=(kb == 0), stop=(kb == nkb - 1))),
                        reads=[("PT", pt), "ones"], writes=[("ps", bd)])
                    S.op("pe", (lambda: nc.tensor.matmul(
                        P.banks[bo][:, :512], lhsT=Vb[:, a, kb, :], rhs=PT[:, pt, :], start=(kb == 0),
                        stop=(kb == nkb - 1))),
                        reads=[("PT", pt), ("V", a)], writes=[("ps", bo)])
                pend_pv = pv
            if pend_pv is not None:
                pend_pv()
            if stop_after in ('D1', 'D2', 'D3'):
                continue
            S.op("act", (lambda bd=bd: nc.scalar.copy(out=rden[:], in_=P.banks[bd][:, :512])),
                 reads=[("ps", bd)], writes=["rden"])
            S.op("dve", lambda: nc.vector.reciprocal(out=rden[:], in_=rden[:]), reads=["rden"], writes=["rden"])
            S.op("dve", (lambda bo=bo, a=a: nc.vector.tensor_tensor(out=ob[:, a, :], in0=P.banks[bo][:, :512], in1=rden[:],
                                                                    op=ALU.mult)),
                 reads=[("ps", bo), "rden"], writes=[("ob", a)])
            P.store(o_d[i, kv], ob[:, a, :], [("ob", a)], lane=f"o{a}", batch=False, eng="pool")
    return P.finish()


def run_l2b(pr):
    nc = get_nc("l2b", build_l2b)
    q, k, v, qi, ki, wi = pr["q"], pr["k"], pr["v"], pr["qi"], pr["ki"], pr["wi"]
    kfull = np.ascontiguousarray(k.reshape(4, 128, L))
    vfull = np.ascontiguousarray(v.reshape(4, 128, 64, 128).transpose(0, 3, 2, 1))
    ki2 = np.ascontiguousarray(np.concatenate([ki, ki], axis=0))
    common = {"ki": ki2, "k": kfull, "v": vfull, "ident": np.eye(128, dtype=np.float32)}
    maps = []
    for core in range(NCORE):
        qb = [8 * i + core for i in range(NQB)]
        qc = np.stack([q[:, :, b * 128:(b + 1) * 128].transpose(1, 0, 2) for b in qb])
        qic = np.stack([qi[:, :, b * 128:(b + 1) * 128].transpose(1, 0, 2) for b in qb])
        wic = np.stack([wi[:, b * 128:(b + 1) * 128].T for b in qb]).astype(np.float32)
        cm = np.zeros((128, 1024), np.float32)
        kpos = np.arange(1024)[None, :]
        qpos = (128 * core + np.arange(128))[:, None]
        cm[kpos > qpos] = -1.0e30
        m = dict(common)
        m.update({"q": np.ascontiguousarray(qc), "qi": np.ascontiguousarray(qic), "wi": np.ascontiguousarray(wic),
                  "cmask": cm})
        maps.append(m)
    outs = run(nc, maps)
    o_fm = np.empty((16, 128, L), outs[0]["o"].dtype)
    for core in range(NCORE):
        o = outs[core]["o"].reshape(NQB, 4, 128, 4, 128)
        for i in range(NQB):
            b = 8 * i + core
            for kv in range(4):
                for h in range(4):
                    o_fm[4 * kv + h, :, b * 128:(b + 1) * 128] = o[i, kv, :, h, :]
    return o_fm


def build_tail(glu, emit_u=False):
    P = Prog()
    nc, S = P.nc, P.S
    N = SEG
    xin_d = P.dram_in("xin", [2, DC, 128, N])
    a_d = P.dram_in("a", [2, DC, 128, N], BF16)
    gains_d = P.dram_in("gains", [128, 5 * DC])
    if emit_u:
        u_d = P.dram_out("u", [2, DC, 128, N], BF16)
    M = 2 * D if glu else D
    w_mix = P.dram_in("w_mix", [M // 512, 128, 8192])
    w_up = P.dram_in("w_up", [16, 128, 8192])
    w_down = P.dram_in("w_down", [16, 128, 8192])
    out_d = P.dram_out("out", [2, DC, 128, N])

    P.consts()
    xin = P.sb("xin", [128, DC, N], F32)
    xn = P.sb("xn", [128, DC, N], BF16)
    tmpA = P.sb("tmpA", [128, DC, N], F32)
    hbuf = P.sb("hbuf", [128, 64, N], BF16)
    rstd = P.sb("rstd", [128, N], F32)
    tmp2 = P.sb("tmp2", [128, 4, N], F32)
    gains = P.sb("gains_sb", [128, 5 * DC], F32)
    P.load(gains[:], gains_d[:, :], "gains")
    for t in range(2):
        for c in range(DC):
            P.load(xin[:, c, :], xin_d[t, c], ("xin", c), lane=f"xin{t}")
            P.load(xn[:, c, :], a_d[t, c], ("xn", c), lane=f"a{t}")
        if glu:
            groups = [[(256 * g, 256), (2048 + 256 * g, 256)] for g in range(8)]

            def evac(gi, bks):
                for j in range(2):
                    c = gi * 2 + j
                    ba, bg = bks[j], bks[2 + j]
                    S.op("act", (lambda bg=bg, j=j: nc.scalar.activation(out=tmp2[:, j, :], in_=P.banks[bg][:, :N],
                                                                         func=AF.Sigmoid)),
                         reads=[("ps", bg)], writes=[("tmp2", j)])
                    S.op("dve", (lambda ba=ba, c=c, j=j: nc.vector.tensor_tensor(
                        out=tmpA[:, c, :], in0=P.banks[ba][:, :N], in1=tmp2[:, j, :], op=ALU.mult)),
                        reads=[("ps", ba), ("tmp2", j)], writes=[("tmpA", c)])
            P.linear(w_mix, D, M, lambda kk: xn[:, kk, :], lambda kk: [("xn", kk)], N, evac, col_groups=groups, pre=True)
        else:
            def evac(gi, bks):
                for mb in range(4):
                    c = gi * 4 + mb
                    S.op("act", (lambda b=bks[mb], c=c: nc.scalar.copy(out=tmpA[:, c, :], in_=P.banks[b][:, :N])),
                         reads=[("ps", bks[mb])], writes=[("tmpA", c)])
            P.linear(w_mix, D, M, lambda kk: xn[:, kk, :], lambda kk: [("xn", kk)], N, evac, pre=True)
        P.rms_rstd(tmpA, "tmpA", 0, N, hbuf, "h", rstd, "rstd")
        P.norm_add(tmpA, "tmpA", N, gains, DC, rstd, "rstd", xin, "xin", 0, tmp2, "tmp2")
        P.mlp(xin, "xin", 0, N, gains, 2 * DC, 3 * DC, w_up, w_down, xn, "xn", hbuf, tmpA, rstd, tmp2)
        for c in range(DC):
            P.store(out_d[t, c], xin[:, c, :], [("xin", c)], lane=f"out{t}")
        if emit_u:
            P.rms_rstd(xin, "xin", 0, N, hbuf, "h", rstd, "rstd")
            P.norm_apply(xin, "xin", 0, N, gains, 4 * DC, rstd, "rstd", xn, "xn", 0)
            for c in range(DC):
                P.store(u_d[t, c], xn[:, c, :], [("xn", c)], lane=f"u{t}")
    return P.finish()


def run_tail(glu, a_fm, x_fm, inp, layer, w_mix, gnext=None):
    emit_u = gnext is not None
    nc = get_nc(("tail", glu, emit_u), lambda: build_tail(glu, emit_u))
    g5 = np.zeros((128, 5 * DC), np.float32)
    g5[:, :4 * DC] = gains_layout(inp["norm_gains"][layer])
    if emit_u:
        g5[:, 4 * DC:] = col_layout(gnext)
    common = {"gains": g5, "w_mix": slotify(np.asarray(w_mix, dtype=np.float32), GLU_GROUPS if glu else None),
              "w_up": slot_cached(inp, "mlp_w_up", layer), "w_down": slot_cached(inp, "mlp_w_down", layer)}
    maps = []
    for core in range(NCORE):
        m = dict(common)
        m.update({"xin": seg_fm(x_fm, core), "a": seg_fm(a_fm, core)})
        maps.append(m)
    outs = run(nc, maps)
    if emit_u:
        return gather_fm(outs), gather_fm(outs, "u")
    return gather_fm(outs)


TWO_PI = 6.283185307179586
NTT = L // 128


def build_l3b(ntiles=NTT, stage=9):
    P = Prog()
    P.bank_list = [4, 5, 6, 7]
    nc, S = P.nc, P.S
    u_d = P.dram_in("u", [2, 128, L], BF16)
    lam_d = P.dram_in("lam", [128, 3, 8])
    bblk_d = P.dram_in("bblk", [128, 2, 2, 512])
    cpad_d = P.dram_in("cpad", [128, 8, 2, 128])
    dsk_d = P.dram_in("dskip", [128, 2])
    tri_d = P.dram_in("tri", [128, 128])
    id_d = P.dram_in("ident", [128, 128])
    y_d = P.dram_out("y", [2, 128, L], BF16)

    lam = P.sb("lam", [128, 3, 8], F32)
    sc = P.sb("sc", [128, 40, 8], F32)
    ki32 = P.sb("ki32", [128, 8], mybir.dt.int32)
    bblkf = P.sb("bblkf", [128, 2, 2, 512], F32)
    bblk = P.sb("bblk_sb", [128, 2, 2, 512], BF16)
    cpadf = P.sb("cpadf", [128, 8, 2, 128], F32)
    cpad = P.sb("cpad_sb", [128, 8, 2, 128], BF16)
    dsk = P.sb("dsk", [128, 2], F32)
    trif = P.sb("trif", [128, 128], F32)
    tri = P.sb("tri_sb", [128, 128], BF16)
    ident = P.sb("ident_sb", [128, 128], F32)
    T2 = P.sb("T2", [128, 2, 8, 128], F32)
    T1s = P.sb("T1s", [128, 2, 8, 128], F32)
    T1 = P.sb("T1", [128, 2, 8, 128], F32)
    tmpp = P.sb("tmpp", [128, 2, 128], F32)
    usb = P.sb("usb", [128, 2, 2, 1024], BF16)
    ysb = P.sb("ysb", [128, 2, 2, 1024], BF16)
    Vb = P.sb("Vb", [128, 2, 2, 1024], BF16)
    Wp = P.sb("Wp", [128, 2, 8, 128], F32)
    hb = P.sb("hb", [128, 2, 8, 128], BF16)
    mm = P.sb("mm", [128, 4, 512], F32)
    gt = P.sb("gt", [128, 4, 128], F32)
    ahp = P.sb("ahp", [128, 2, 8], F32)
    t6 = P.sb("t6", [128, 6, 8], F32)

    P.load(lam[:], lam_d[:, :, :], "lam")
    P.load(bblkf[:], bblk_d[:, :, :, :], "bblkf")
    P.load(cpadf[:], cpad_d[:, :, :, :], "cpadf")
    P.load(dsk[:], dsk_d[:, :], "dsk")
    P.load(trif[:], tri_d[:, :], "trif")
    P.load(ident[:], id_d[:, :], "ident")
    S.op("dve", lambda: nc.vector.tensor_copy(out=bblk[:], in_=bblkf[:]), reads=["bblkf"], writes=["bblk"])
    S.op("dve", lambda: nc.vector.tensor_copy(out=tri[:], in_=trif[:]), reads=["trif"], writes=["tri"])
    S.op("dve", lambda: nc.vector.tensor_copy(out=cpad[:, :, 0, :], in_=cpadf[:, :, 0, :]), reads=["cpadf"], writes=["cpad0"])
    S.op("dve", lambda: nc.vector.tensor_scalar(out=cpad[:, :, 1, :], in0=cpadf[:, :, 1, :], scalar1=-1.0, scalar2=None,
                                                op0=ALU.mult), reads=["cpadf"], writes=["cpad1"])

    def R(i):
        return sc[:, i, :]

    def tt(o, a, b, op):
        S.op("dve", lambda: nc.vector.tensor_tensor(out=R(o), in0=R(a), in1=R(b), op=op),
             reads=[("sc", a), ("sc", b)], writes=[("sc", o)])

    def ts(o, a, s1, op0, s2=None, op1=None):
        if op1 is None:
            S.op("dve", lambda: nc.vector.tensor_scalar(out=R(o), in0=R(a), scalar1=s1, scalar2=None, op0=op0),
                 reads=[("sc", a)], writes=[("sc", o)])
        else:
            S.op("dve", lambda: nc.vector.tensor_scalar(out=R(o), in0=R(a), scalar1=s1, scalar2=s2, op0=op0, op1=op1),
                 reads=[("sc", a)], writes=[("sc", o)])

    def act(o, a, func, scale=1.0):
        S.op("act", lambda: nc.scalar.activation(out=R(o), in_=R(a), func=func, scale=scale),
             reads=[("sc", a)], writes=[("sc", o)])

    def wrap_pi(o, a, t1, t2):
        ts(t1, a, 1.0 / TWO_PI, ALU.mult)
        S.op("dve", lambda: nc.vector.tensor_copy(out=ki32[:], in_=R(t1)), reads=[("sc", t1)], writes=["ki32"])
        S.op("dve", lambda: nc.vector.tensor_copy(out=R(t1), in_=ki32[:]), reads=["ki32"], writes=[("sc", t1)])
        S.op("dve", lambda: nc.vector.scalar_tensor_tensor(out=R(o), in0=R(t1), scalar=-TWO_PI, in1=R(a),
                                                           op0=ALU.mult, op1=ALU.add),
             reads=[("sc", t1), ("sc", a)], writes=[("sc", o)])
        ts(t2, o, 3.141592653589793, ALU.is_gt, -TWO_PI, ALU.mult)
        tt(o, o, t2, ALU.add)
        ts(t2, o, -3.141592653589793, ALU.is_lt, TWO_PI, ALU.mult)
        tt(o, o, t2, ALU.add)

    S.op("dve", lambda: nc.vector.tensor_copy(out=sc[:, 0:3, :], in_=lam[:]), reads=["lam"],
         writes=[("sc", 0), ("sc", 1), ("sc", 2)])
    act(2, 2, AF.Exp)
    tt(3, 0, 2, ALU.mult)
    tt(4, 1, 2, ALU.mult)
    act(5, 3, AF.Exp)
    wrap_pi(12, 4, 13, 14)
    act(15, 12, AF.Sin)
    ts(16, 4, 1.5707963267948966, ALU.add)
    wrap_pi(17, 16, 13, 14)
    act(18, 17, AF.Sin)
    tt(6, 5, 18, ALU.mult)
    tt(7, 5, 15, ALU.mult)
    act(19, 3, AF.Exp, scale=-1.0)
    tt(8, 19, 18, ALU.mult)
    tt(9, 19, 15, ALU.mult)
    ts(9, 9, -1.0, ALU.mult)
    ts(20, 6, -1.0, ALU.add)
    tt(21, 0, 0, ALU.mult)
    tt(22, 1, 1, ALU.mult)
    tt(21, 21, 22, ALU.add)
    S.op("dve", lambda: nc.vector.reciprocal(out=R(21), in_=R(21)), reads=[("sc", 21)], writes=[("sc", 21)])
    tt(22, 20, 0, ALU.mult)
    tt(23, 7, 1, ALU.mult)
    tt(22, 22, 23, ALU.add)
    tt(10, 22, 21, ALU.mult)
    tt(22, 7, 0, ALU.mult)
    tt(23, 20, 1, ALU.mult)
    tt(22, 22, 23, ALU.subtract)
    tt(11, 22, 21, ALU.mult)

    def power_table(T, base_r, base_i, init_r, init_i, pr, pi, q1, q2):
        tt(pr, base_r, base_r, ALU.max)
        tt(pi, base_i, base_i, ALU.max)
        for j in range(8):
            if init_r is None:
                S.op("dve", (lambda j=j: nc.vector.memset(T[:, 0, j, 0:1], 1.0)), writes=[("T", id(T), j)])
                S.op("dve", (lambda j=j: nc.vector.memset(T[:, 1, j, 0:1], 0.0)), writes=[("T", id(T), j)])
            else:
                S.op("dve", (lambda j=j: nc.vector.tensor_copy(out=T[:, 0, j, 0:1], in_=sc[:, init_r, j:j + 1])),
                     reads=[("sc", init_r)], writes=[("T", id(T), j)])
                S.op("dve", (lambda j=j: nc.vector.tensor_copy(out=T[:, 1, j, 0:1], in_=sc[:, init_i, j:j + 1])),
                     reads=[("sc", init_i)], writes=[("T", id(T), j)])
        for k in range(7):
            w = 1 << k
            for j in range(8):
                key = ("T", id(T), j)
                S.op("dve", (lambda j=j, w=w: nc.vector.tensor_scalar(
                    out=tmpp[:, 0, :w], in0=T[:, 1, j, 0:w], scalar1=sc[:, pi, j:j + 1], scalar2=None, op0=ALU.mult)),
                    reads=[key, ("sc", pi)], writes=[("tmpp", 0)])
                S.op("dve", (lambda j=j, w=w: nc.vector.scalar_tensor_tensor(
                    out=T[:, 0, j, w:2 * w], in0=T[:, 0, j, 0:w], scalar=sc[:, pr, j:j + 1], in1=tmpp[:, 0, :w],
                    op0=ALU.mult, op1=ALU.subtract)),
                    reads=[key, ("sc", pr), ("tmpp", 0)], writes=[key])
                S.op("dve", (lambda j=j, w=w: nc.vector.tensor_scalar(
                    out=tmpp[:, 1, :w], in0=T[:, 1, j, 0:w], scalar1=sc[:, pr, j:j + 1], scalar2=None, op0=ALU.mult)),
                    reads=[key, ("sc", pr)], writes=[("tmpp", 1)])
                S.op("dve", (lambda j=j, w=w: nc.vector.scalar_tensor_tensor(
                    out=T[:, 1, j, w:2 * w], in0=T[:, 0, j, 0:w], scalar=sc[:, pi, j:j + 1], in1=tmpp[:, 1, :w],
                    op0=ALU.mult, op1=ALU.add)),
                    reads=[key, ("sc", pi), ("tmpp", 1)], writes=[key])
            tt(q1, pr, pr, ALU.mult)
            tt(q2, pi, pi, ALU.mult)
            tt(q2, q1, q2, ALU.subtract)
            tt(q1, pr, pi, ALU.mult)
            ts(pi, q1, 2.0, ALU.mult)
            tt(pr, q2, q2, ALU.max)

    power_table(T2, 6, 7, None, None, 24, 25, 26, 27)
    power_table(T1s, 8, 9, 10, 11, 28, 29, 26, 27)
    for ri in range(2):
        for j4 in range(2):
            b = P.bank()
            for jj in range(4):
                j = j4 * 4 + jj
                S.op("pe", (lambda b=b, jj=jj, j=j, ri=ri: nc.tensor.matmul(
                    P.banks[b][:, jj * 128:(jj + 1) * 128], lhsT=T1s[:, ri, j, :], rhs=ident[:], start=True, stop=True)),
                    reads=[("T", id(T1s), j), "ident"], writes=[("ps", b)])
            S.op("act", (lambda b=b, ri=ri, j4=j4: nc.scalar.copy(
                out=T1[:, ri, j4 * 4:(j4 + 1) * 4, :], in_=P.banks[b][:, :512].rearrange("p (j n) -> p j n", n=128))),
                reads=[("ps", b)], writes=[("T1", ri, j4)])
    S.op("dve", lambda: nc.vector.memset(ahp[:], 0.0), writes=["ahp"])
    T2keys = [("T", id(T2), j) for j in range(8)]

    for it in range(ntiles):
        big = it // 8
        ub = big % 2
        if it % 8 == 0:
            for fc in range(2):
                P.load(usb[:, ub, fc, :], u_d[fc, :, big * 1024:(big + 1) * 1024], ("u", ub, fc), lane=f"u{big}")
        tcol = (it % 8) * 128
        vb = it % 2
        xb = {}
        for fc in range(2):
            for ri in range(2):
                b = P.bank()
                xb[(fc, ri)] = b
                S.op("pe", (lambda b=b, fc=fc, ri=ri, ub=ub, tcol=tcol: nc.tensor.matmul(
                    P.banks[b][:, :512], lhsT=usb[:, ub, fc, tcol:tcol + 128], rhs=bblk[:, fc, ri, :],
                    start=True, stop=True)),
                    reads=[("u", ub, fc), "bblk"], writes=[("ps", b)])
        for fc in range(2):
            br_, bi_ = xb[(fc, 0)], xb[(fc, 1)]
            t1r = T1[:, 0, fc * 4:(fc + 1) * 4, :]
            t1i = T1[:, 1, fc * 4:(fc + 1) * 4, :]
            rk = [("T1", 0, fc), ("T1", 1, fc)]

            def pmul(o, b, tab):
                S.op("dve", (lambda o=o, b=b, tab=tab: nc.vector.tensor_tensor(
                    out=mm[:, o, :].rearrange("p (j n) -> p j n", n=128),
                    in0=P.banks[b][:, :512].rearrange("p (j n) -> p j n", n=128), in1=tab, op=ALU.mult)),
                    reads=[("ps", b)] + rk, writes=[("mm", o)])
            pmul(0, br_, t1r)
            pmul(1, bi_, t1i)
            S.op("dve", (lambda fc=fc, vb=vb: nc.vector.tensor_tensor(
                out=Vb[:, vb, 0, fc * 512:(fc + 1) * 512], in0=mm[:, 0, :], in1=mm[:, 1, :], op=ALU.subtract)),
                reads=[("mm", 0), ("mm", 1)], writes=[("V", vb, 0, fc)])
            pmul(2, br_, t1i)
            pmul(3, bi_, t1r)
            S.op("dve", (lambda fc=fc, vb=vb: nc.vector.tensor_tensor(
                out=Vb[:, vb, 1, fc * 512:(fc + 1) * 512], in0=mm[:, 2, :], in1=mm[:, 3, :], op=ALU.add)),
                reads=[("mm", 2), ("mm", 3)], writes=[("V", vb, 1, fc)])
        if stage < 2:
            continue
        for ri in range(2):
            for j4 in range(2):
                b = P.bank()
                for jj in range(4):
                    j = j4 * 4 + jj
                    S.op("pe", (lambda b=b, jj=jj, j=j, ri=ri, vb=vb: nc.tensor.matmul(
                        P.banks[b][:, jj * 128:(jj + 1) * 128], lhsT=Vb[:, vb, ri, j * 128:(j + 1) * 128], rhs=tri[:],
                        start=True, stop=True)),
                        reads=[("V", vb, ri, j // 4), "tri"], writes=[("ps", b)])
                for jj in range(4):
                    j = j4 * 4 + jj
                    S.op("dve", (lambda b=b, jj=jj, j=j, ri=ri: nc.vector.tensor_scalar(
                        out=Wp[:, ri, j, :], in0=P.banks[b][:, jj * 128:(jj + 1) * 128], scalar1=ahp[:, ri, j:j + 1],
                        scalar2=None, op0=ALU.add)),
                        reads=[("ps", b), "ahp"], writes=[("Wp", ri, j)])
        Wkeys = [("Wp", ri, j) for ri in range(2) for j in range(8)]
        if stage < 3:
            continue
        wl_r = Wp[:, 0, :, 127]
        wl_i = Wp[:, 1, :, 127]
        S.op("dve", lambda: nc.vector.tensor_tensor(out=t6[:, 0, :], in0=wl_r, in1=sc[:, 24, :], op=ALU.mult),
             reads=Wkeys + [("sc", 24)], writes=[("t6", 0)])
        S.op("dve", lambda: nc.vector.tensor_tensor(out=t6[:, 1, :], in0=wl_i, in1=sc[:, 25, :], op=ALU.mult),
             reads=Wkeys + [("sc", 25)], writes=[("t6", 1)])
        S.op("dve", lambda: nc.vector.tensor_tensor(out=t6[:, 2, :], in0=wl_r, in1=sc[:, 25, :], op=ALU.mult),
             reads=Wkeys + [("sc", 25)], writes=[("t6", 2)])
        S.op("dve", lambda: nc.vector.tensor_tensor(out=t6[:, 3, :], in0=wl_i, in1=sc[:, 24, :], op=ALU.mult),
             reads=Wkeys + [("sc", 24)], writes=[("t6", 3)])
        S.op("dve", lambda: nc.vector.tensor_tensor(out=ahp[:, 0, :], in0=t6[:, 0, :], in1=t6[:, 1, :], op=ALU.subtract),
             reads=[("t6", 0), ("t6", 1)] + Wkeys, writes=["ahp"])
        S.op("dve", lambda: nc.vector.tensor_tensor(out=ahp[:, 1, :], in0=t6[:, 2, :], in1=t6[:, 3, :], op=ALU.add),
             reads=[("t6", 2), ("t6", 3)], writes=["ahp"])
        if stage < 4:
            continue
        for hf in range(2):
            js = slice(hf * 4, hf * 4 + 4)
            wk = [("Wp", ri, j) for ri in range(2) for j in range(hf * 4, hf * 4 + 4)]

            def hmul(o, ri, ti, hf=hf, js=js, wk=wk):
                S.op("dve", (lambda: nc.vector.tensor_tensor(
                    out=mm[:, o, :].rearrange("p (j n) -> p j n", n=128), in0=Wp[:, ri, js, :], in1=T2[:, ti, js, :],
                    op=ALU.mult)),
                    reads=wk + T2keys, writes=[("mm", o)])
            hmul(0, 0, 0)
            hmul(1, 1, 1)
            S.op("dve", (lambda js=js: nc.vector.tensor_tensor(
                out=hb[:, 0, js, :], in0=mm[:, 0, :].rearrange("p (j n) -> p j n", n=128),
                in1=mm[:, 1, :].rearrange("p (j n) -> p j n", n=128), op=ALU.subtract)),
                reads=[("mm", 0), ("mm", 1)], writes=[("hb", 0, hf)])
            hmul(2, 0, 1)
            hmul(3, 1, 0)
            S.op("dve", (lambda js=js: nc.vector.tensor_tensor(
                out=hb[:, 1, js, :], in0=mm[:, 2, :].rearrange("p (j n) -> p j n", n=128),
                in1=mm[:, 3, :].rearrange("p (j n) -> p j n", n=128), op=ALU.add)),
                reads=[("mm", 2), ("mm", 3)], writes=[("hb", 1, hf)])
        if stage < 5:
            continue
        for fc in range(2):
            b = P.bank()
            n = 0
            for jj in range(4):
                j = fc * 4 + jj
                for ri in range(2):
                    S.op("pe", (lambda b=b, j=j, ri=ri, n=n: nc.tensor.matmul(
                        P.banks[b][:, :128], lhsT=cpad[:, j, ri, :], rhs=hb[:, ri, j, :], start=(n == 0), stop=(n == 7))),
                        reads=[("hb", ri, fc), f"cpad{ri}"], writes=[("ps", b)])
                    n += 1
            S.op("dve", (lambda b=b, fc=fc, ub=ub, tcol=tcol: nc.vector.scalar_tensor_tensor(
                out=gt[:, 0, :], in0=usb[:, ub, fc, tcol:tcol + 128], scalar=dsk[:, fc:fc + 1], in1=P.banks[b][:, :128],
                op0=ALU.mult, op1=ALU.add)),
                reads=[("ps", b), ("u", ub, fc), "dsk"], writes=[("gt", 0)])
            S.op("act", lambda: nc.scalar.activation(out=gt[:, 1, :], in_=gt[:, 0, :], func=AF.Square),
                 reads=[("gt", 0)], writes=[("gt", 1)])
            S.op("dve", lambda: nc.vector.tensor_scalar(out=gt[:, 1, :], in0=gt[:, 1, :], scalar1=0.044715, scalar2=1.0,
                                                        op0=ALU.mult, op1=ALU.add),
                 reads=[("gt", 1)], writes=[("gt", 1)])
            S.op("dve", lambda: nc.vector.tensor_tensor(out=gt[:, 2, :], in0=gt[:, 1, :], in1=gt[:, 0, :], op=ALU.mult),
                 reads=[("gt", 1), ("gt", 0)], writes=[("gt", 2)])
            S.op("act", lambda: nc.scalar.activation(out=gt[:, 3, :], in_=gt[:, 2, :], func=AF.Sigmoid, scale=1.5957691216),
                 reads=[("gt", 2)], writes=[("gt", 3)])
            S.op("dve", (lambda fc=fc, ub=ub, tcol=tcol: nc.vector.tensor_tensor(
                out=ysb[:, ub, fc, tcol:tcol + 128], in0=gt[:, 0, :], in1=gt[:, 3, :], op=ALU.mult)),
                reads=[("gt", 0), ("gt", 3)], writes=[("y", ub)])
        if it % 8 == 7 or it == ntiles - 1:
            for fc in range(2):
                P.store(y_d[fc, :, big * 1024:(big + 1) * 1024], ysb[:, ub, fc, :], [("y", ub)], lane=f"y{ub}", batch=False, eng="pool")
    return P.finish()


def run_l3b(u_fm, inp, ntiles=NTT, stage=9):
    nc = get_nc(("l3b", ntiles, stage), lambda: build_l3b(ntiles, stage))
    lr, li, ldt = inp["ssm_lambda_re"][0], inp["ssm_lambda_im"][0], inp["ssm_log_dt"][0]
    bre, bim = inp["ssm_b_re"][0], inp["ssm_b_im"][0]
    cre, cim = inp["ssm_c_re"][0], inp["ssm_c_im"][0]
    tri = np.triu(np.ones((128, 128), np.float32))
    maps = []
    for core in range(NCORE):
        g0 = 16 * core
        lam = np.zeros((128, 3, 8), np.float32)
        bblk = np.zeros((128, 2, 2, 512), np.float32)
        cpad = np.zeros((128, 8, 2, 128), np.float32)
        for j in range(8):
            for gp in range(2):
                g = g0 + 2 * j + gp
                rows = slice(gp * 64, gp * 64 + 64)
                lam[rows, 0, j] = lr[g]
                lam[rows, 1, j] = li[g]
                lam[rows, 2, j] = ldt[g]
                gl = (2 * j + gp) % 8
                fc = j // 4
                cpad[rows, j, 0, gl * 16:gl * 16 + 16] = cre[g].T
                cpad[rows, j, 1, gl * 16:gl * 16 + 16] = cim[g].T
                bblk[gl * 16:gl * 16 + 16, fc, 0, gl * 64:gl * 64 + 64] = bre[g].T
                bblk[gl * 16:gl * 16 + 16, fc, 1, gl * 64:gl * 64 + 64] = bim[g].T
        dsk = np.ascontiguousarray(inp["ssm_d"][0][256 * core:256 * core + 256].reshape(2, 128).T)
        maps.append({"u": np.ascontiguousarray(u_fm[2 * core:2 * core + 2]), "lam": lam, "bblk": bblk, "cpad": cpad,
                     "dskip": dsk, "tri": tri, "ident": np.eye(128, dtype=np.float32)})
    outs = run(nc, maps)
    y = np.empty((16, 128, L), outs[0]["y"].dtype)
    for core in range(NCORE):
        y[2 * core:2 * core + 2] = outs[core]["y"]
    return y


def seg_fm(a_fm, core):
    return np.ascontiguousarray(np.stack([a_fm[:, :, s * SEG:(s + 1) * SEG] for s in seg_of(core)]))


def gather_fm(outs, name="out"):
    C = outs[0][name].shape[1]
    full = np.empty((C, 128, L), outs[0][name].dtype)
    for core in range(NCORE):
        for i, s in enumerate(seg_of(core)):
            full[:, :, s * SEG:(s + 1) * SEG] = outs[core][name][i]
    return full


GLU_GROUPS = [[(256 * g, 256), (2048 + 256 * g, 256)] for g in range(8)]


def slotify(W, groups=None):
    K, M = W.shape
    if groups is None:
        groups = [[(m0, 512)] for m0 in range(0, M, 512)]
    nk = K // 2048
    out = np.empty((len(groups) * nk, 128, 16, 512), np.float32)
    for gi, grp in enumerate(groups):
        cols = np.concatenate([np.arange(m0, m0 + n) for (m0, n) in grp])
        sub = W[:, cols] if len(grp) > 1 else W[:, grp[0][0]:grp[0][0] + 512]
        sub = sub.reshape(nk, 16, 128, 512).transpose(0, 2, 1, 3)
        out[gi * nk:(gi + 1) * nk] = sub
    return out.reshape(len(groups) * nk, 128, 8192)


_SLOT_CACHE = {}


def slot_cached(inp, name, idx, groups=None):
    key = (name, idx, groups is not None)
    if key not in _SLOT_CACHE:
        _SLOT_CACHE[key] = slotify(np.asarray(inp[name][idx], dtype=np.float32), groups)
    return _SLOT_CACHE[key]


def gains_layout(norm_gains_i):
    return np.ascontiguousarray(np.concatenate([col_layout(norm_gains_i[j]) for j in range(4)], axis=1))


_NC_CACHE = {}


def get_nc(name, builder):
    if name not in _NC_CACHE:
        _NC_CACHE[name] = builder()
    return _NC_CACHE[name]


def run_layer0(x_fm, inp):
    nc = get_nc("l0", build_layer0)
    vecs = np.zeros((128, 6 * DC), np.float32)
    vecs[:, 0:DC] = col_layout(inp["conv_b_in"][0][:D])
    vecs[:, DC:2 * DC] = col_layout(inp["conv_b_in"][0][D:])
    vecs[:, 2 * DC:3 * DC] = col_layout(inp["conv_b_dw"][0])
    vecs[:, 3 * DC:4 * DC] = col_layout(inp["conv_ln_g"][0])
    vecs[:, 4 * DC:5 * DC] = col_layout(inp["conv_ln_b"][0])
    wdw = np.ascontiguousarray(inp["conv_w_dw"][0].T.reshape(DC, 128, 31).transpose(1, 0, 2))
    common = {"gains": gains_layout(inp["norm_gains"][0]), "vecs": vecs, "wdw": wdw,
              "ident": np.eye(128, dtype=np.float32),
              "w_in": slot_cached(inp, "conv_w_in", 0, GLU_GROUPS), "w_out": slot_cached(inp, "conv_w_out", 0),
              "w_up": slot_cached(inp, "mlp_w_up", 0), "w_down": slot_cached(inp, "mlp_w_down", 0)}
    maps = []
    for core in range(NCORE):
        xh = np.zeros((DC, 128, 2 * HALO), np.float32)
        hm = np.zeros((128, 2 * HALO), np.float32)
        for i, s in enumerate(seg_of(core)):
            if s > 0:
                xh[:, :, i * HALO:(i + 1) * HALO] = x_fm[:, :, s * SEG - HALO:s * SEG]
                hm[:, i * HALO:(i + 1) * HALO] = 1.0
        m = dict(common)
        m.update({"xin": seg_fm(x_fm, core), "xh": xh, "hmask": hm})
        maps.append(m)
    return gather_fm(run(nc, maps))


def halo_fm(x_fm, core, halo=HALO):
    xh = np.zeros((x_fm.shape[0], 128, 2 * halo), np.float32)
    for i, s in enumerate(seg_of(core)):
        if s > 0:
            xh[:, :, i * halo:(i + 1) * halo] = x_fm[:, :, s * SEG - halo:s * SEG]
    return xh


def run_layer1(x_fm, inp):
    nc = get_nc("l1", build_layer1)
    common = {"gains": gains_layout(inp["norm_gains"][1]), "pscale": col_layout(inp["pool_scale"][0]),
              "w_pool": np.ascontiguousarray(inp["pool_w"][0].reshape(D, 512)),
              "w_up": slot_cached(inp, "mlp_w_up", 1), "w_down": slot_cached(inp, "mlp_w_down", 1)}
    maps = []
    for core in range(NCORE):
        rdiv = np.empty((2, 128, 4, SEG), np.float32)
        for i, s in enumerate(seg_of(core)):
            tpos = np.arange(s * SEG, (s + 1) * SEG) + 1
            for g, w in enumerate(POOL_W):
                rdiv[i, :, g, :] = (1.0 / np.minimum(tpos, w)).astype(np.float32)[None, :]
        m = dict(common)
        m.update({"xin": seg_fm(x_fm, core), "xh": halo_fm(x_fm, core, 16), "rdiv": rdiv})
        maps.append(m)
    return gather_fm(run(nc, maps))


def kernel(**inputs):
    _SLOT_CACHE.clear()
    inp = {k: np.asarray(v) for k, v in inputs.items()}
    x_fm = to_fm(np.ascontiguousarray(inp["x"][0], dtype=np.float32))
    r0 = run_layer0(x_fm, inp)
    r1 = run_layer1(r0, inp)
    pr = run_l2a(r1, inp)
    o_fm = run_l2b(pr)
    r2, u_fm = run_tail(False, o_fm, r1, inp, 2, inp["attn_w_out"][0], gnext=inp["norm_gains"][3][0])
    y_fm = run_l3b(u_fm, inp)
    r3 = run_tail(True, y_fm, r2, inp, 3, inp["ssm_w_glu"][0])
    return from_fm(r3)[None].astype(np.float32)
```
